# Optimizing a Trainium2 kernel written in Bass

```python
import math
import jax, jax.numpy as jnp
from jax import lax
import numpy as np

D_MODEL = 1024
BATCH = 8
SEQ = 4096
DEPTH = 1

GRID_W = 64
CTX_LEN = 256
EPS = 1e-6
MIX_WIDTH = D_MODEL
MLA_HEADS = 8
MLA_NOPE = 64
MLA_ROPE = 32
MLA_V = 64
MLA_Q_RANK = 256
MLA_KV_RANK = 128
MLA_WIDTH = MLA_HEADS * MLA_V
LRU_WIDTH = MIX_WIDTH - MLA_WIDTH
LRU_BLOCKS = 8
LRU_BLOCK = LRU_WIDTH // LRU_BLOCKS
CONV_W = 4
CONV_LEFT = 2
RG_C = 8.0
ROPE_BASE = 10000.0
ROPE_AXIS = MLA_ROPE // 2
ATTN_SCALE = (MLA_NOPE + MLA_ROPE) ** -0.5
Q_BLOCK = 128
COL_Q = 0
COL_KV = COL_Q + MLA_Q_RANK
COL_KR = COL_KV + MLA_KV_RANK
COL_LRU_X = COL_KR + MLA_ROPE
COL_LRU_G = COL_LRU_X + LRU_WIDTH
IN_COLS = COL_LRU_G + LRU_WIDTH
N_EXPERTS = 64
TOP_K = 8
N_GROUPS = 8
TOPK_GROUPS = 4
EXPERT_FF = 256
SHARED_FF = 256
ROUTED_SCALE = 2.5
DISPATCH_BLOCK = 256

kernel_name = "hymba_mla_rglru_moe_dit_layer"


def rmsnorm(t, g):
    t32 = t.astype(jnp.float32)
    t32 = t32 * lax.rsqrt(jnp.mean(t32 * t32, axis=-1, keepdims=True) + EPS)
    return (t32 * g.astype(jnp.float32)).astype(t.dtype)


def modulate(t, shift, scale):
    return t * (1.0 + scale) + shift


def axial_rope_tables(n_tokens):
    rows = n_tokens // GRID_W
    row = jnp.repeat(jnp.arange(rows, dtype=jnp.float32), GRID_W)
    col = jnp.tile(jnp.arange(GRID_W, dtype=jnp.float32), rows)
    inv_freq = ROPE_BASE ** (-jnp.arange(0, ROPE_AXIS, 2, dtype=jnp.float32) / ROPE_AXIS)
    ang = jnp.stack([row, col], axis=-1)[:, :, None] * inv_freq
    return jnp.cos(ang)[:, None], jnp.sin(ang)[:, None]


def apply_axial_rope(t, cos, sin):
    b_, s_, h_, _ = t.shape
    t32 = t.astype(jnp.float32).reshape(b_, s_, h_, 2, 2, ROPE_AXIS // 2)
    t1, t2 = t32[..., 0, :], t32[..., 1, :]
    out = jnp.stack([t1 * cos - t2 * sin, t1 * sin + t2 * cos], axis=-2)
    return out.reshape(b_, s_, h_, MLA_ROPE).astype(t.dtype)


def mla_q(proj, q_norm_g, w_q_up):
    b_, l_, _ = proj.shape
    q = (rmsnorm(proj[..., COL_Q:COL_KV], q_norm_g) @ w_q_up).reshape(b_, l_, MLA_HEADS, MLA_NOPE + MLA_ROPE)
    return q[..., :MLA_NOPE], q[..., MLA_NOPE:]


def mla_kv(proj, kv_norm_g, w_kv_up):
    b_, l_, _ = proj.shape
    kv = (rmsnorm(proj[..., COL_KV:COL_KR], kv_norm_g) @ w_kv_up).reshape(b_, l_, MLA_HEADS, MLA_NOPE + MLA_V)
    k_rope = proj[..., COL_KR:COL_LRU_X][:, :, None, :]
    return kv[..., :MLA_NOPE], k_rope, kv[..., MLA_NOPE:]


def join_key(k_nope, k_rope):
    return jnp.concatenate([k_nope, jnp.broadcast_to(k_rope, k_nope.shape[:-1] + (MLA_ROPE,))], axis=-1)


def attend(q, k, v):
    s = jnp.einsum('bqhd,bkhd->bhqk', q, k, preferred_element_type=jnp.float32) * ATTN_SCALE
    p = jax.nn.softmax(s, axis=-1)
    return jnp.einsum('bhqk,bkhd->bqhd', p.astype(v.dtype), v)


def short_conv(u, w, b):
    l_ = u.shape[1]
    up = jnp.pad(u, ((0, 0), (CONV_LEFT, CONV_W - 1 - CONV_LEFT), (0, 0)))
    out = b
    for j in range(CONV_W):
        out = out + up[:, j:j + l_] * w[j]
    return out


def block_diag(u, w, b):
    ub = u.reshape(u.shape[:-1] + (LRU_BLOCKS, LRU_BLOCK))
    return jnp.einsum('blnc,ncd->blnd', ub, w).reshape(u.shape) + b


def rglru_coeffs(u, w_a, b_a, w_x, b_x, lam):
    r = jax.nn.sigmoid(block_diag(u, w_a, b_a).astype(jnp.float32))
    i = jax.nn.sigmoid(block_diag(u, w_x, b_x).astype(jnp.float32))
    log_a = -RG_C * r * jax.nn.softplus(-lam.astype(jnp.float32))
    a = jnp.exp(log_a)
    b = jnp.sqrt(-jnp.expm1(2.0 * log_a)) * (i * u.astype(jnp.float32))
    return a, b


def linear_scan(a, b, h0):
    b = b.at[:, 0].add(a[:, 0] * h0)

    def combine(left, right):
        return left[0] * right[0], right[0] * left[1] + right[1]

    return lax.associative_scan(combine, (a, b), axis=1)[1]


def rglru_bidirectional(u_lat, u_ctx, lru_w_a, lru_b_a, lru_w_x, lru_b_x, lru_lambda, need_ctx):
    ys_lat, ys_ctx = [], []
    for d in range(2):
        flip = (lambda t: t) if d == 0 else (lambda t: jnp.flip(t, axis=1))
        params = (lru_w_a[d], lru_b_a[d], lru_w_x[d], lru_b_x[d], lru_lambda[d])
        a_c, b_c = rglru_coeffs(flip(u_ctx), *params)
        h_c = linear_scan(a_c, b_c, jnp.zeros_like(b_c[:, 0]))
        a_l, b_l = rglru_coeffs(flip(u_lat), *params)
        h_l = linear_scan(a_l, b_l, h_c[:, -1])
        ys_lat.append(flip(h_l))
        if need_ctx:
            ys_ctx.append(flip(h_c))
    y_ctx = ys_ctx[0] + ys_ctx[1] if need_ctx else None
    return ys_lat[0] + ys_lat[1], y_ctx


def hybrid_mixer(h_lat, h_ctx, cos, sin, w_in, q_norm_g, w_q_up, kv_norm_g, w_kv_up,
                 conv_w, conv_b, lru_w_a, lru_b_a, lru_w_x, lru_b_x, lru_lambda, need_ctx):
    b_, s_, _ = h_lat.shape
    p_lat = h_lat @ w_in
    p_ctx = h_ctx @ w_in

    qn_l, qr_l = mla_q(p_lat, q_norm_g, w_q_up)
    kn_l, kr_l, v_l = mla_kv(p_lat, kv_norm_g, w_kv_up)
    kn_c, kr_c, v_c = mla_kv(p_ctx, kv_norm_g, w_kv_up)
    q_lat = jnp.concatenate([qn_l, apply_axial_rope(qr_l, cos, sin)], axis=-1)
    k_ctx = join_key(kn_c, kr_c)
    k_all = jnp.concatenate([join_key(kn_l, apply_axial_rope(kr_l, cos, sin)), k_ctx], axis=1)
    v_all = jnp.concatenate([v_l, v_c], axis=1)
    n_blk = s_ // Q_BLOCK
    q_blocks = jnp.moveaxis(q_lat.reshape(b_, n_blk, Q_BLOCK, MLA_HEADS, MLA_NOPE + MLA_ROPE), 1, 0)
    o_lat = lax.map(lambda qb: attend(qb, k_all, v_all), q_blocks)
    o_lat = jnp.moveaxis(o_lat, 0, 1).reshape(b_, s_, MLA_WIDTH)

    u_lat = short_conv(p_lat[..., COL_LRU_X:COL_LRU_G], conv_w, conv_b)
    u_ctx = short_conv(p_ctx[..., COL_LRU_X:COL_LRU_G], conv_w, conv_b)
    r_lat, r_ctx = rglru_bidirectional(u_lat, u_ctx, lru_w_a, lru_b_a, lru_w_x, lru_b_x, lru_lambda, need_ctx)
    g_lat = jax.nn.gelu(p_lat[..., COL_LRU_G:].astype(jnp.float32))
    y_lat = jnp.concatenate([o_lat, (r_lat * g_lat).astype(h_lat.dtype)], axis=-1)

    y_ctx = None
    if need_ctx:
        qn_c, qr_c = mla_q(p_ctx, q_norm_g, w_q_up)
        o_ctx = attend(jnp.concatenate([qn_c, qr_c], axis=-1), k_ctx, v_c).reshape(b_, -1, MLA_WIDTH)
        g_ctx = jax.nn.gelu(p_ctx[..., COL_LRU_G:].astype(jnp.float32))
        y_ctx = jnp.concatenate([o_ctx, (r_ctx * g_ctx).astype(h_ctx.dtype)], axis=-1)
    return y_lat, y_ctx


def swiglu(u, w_gate, w_up, w_down):
    return (jax.nn.silu(u @ w_gate) * (u @ w_up)) @ w_down


def routed_experts(h, eidx, w, exp_w_gate, exp_w_up, exp_w_down):
    t_, _ = h.shape
    n_assign = t_ * TOP_K
    n_blocks = n_assign // DISPATCH_BLOCK + N_EXPERTS
    e_flat = eidx.reshape(-1)
    tok_flat = jnp.repeat(jnp.arange(t_, dtype=jnp.int32), TOP_K)
    order = jnp.argsort(e_flat, stable=True)
    e_sorted = e_flat[order]
    counts = jnp.bincount(e_flat, length=N_EXPERTS)
    starts = jnp.cumsum(counts) - counts
    padded = (counts + DISPATCH_BLOCK - 1) // DISPATCH_BLOCK * DISPATCH_BLOCK
    pad_end = jnp.cumsum(padded)
    dest = pad_end[e_sorted] - padded[e_sorted] + (jnp.arange(n_assign, dtype=jnp.int32) - starts[e_sorted])
    slot_tok = jnp.zeros((n_blocks * DISPATCH_BLOCK,), jnp.int32).at[dest].set(tok_flat[order])
    slot_w = jnp.zeros((n_blocks * DISPATCH_BLOCK,), w.dtype).at[dest].set(w.reshape(-1)[order])
    blk_expert = jnp.minimum(
        jnp.searchsorted(pad_end, jnp.arange(n_blocks, dtype=jnp.int32) * DISPATCH_BLOCK, side='right'),
        N_EXPERTS - 1)

    def body(acc, blk):
        tok_b, w_b, e_b = blk
        y = swiglu(h[tok_b], exp_w_gate[e_b], exp_w_up[e_b], exp_w_down[e_b])
        return acc.at[tok_b].add(y * w_b[:, None]), None

    acc, _ = lax.scan(body, jnp.zeros_like(h),
                      (slot_tok.reshape(n_blocks, DISPATCH_BLOCK),
                       slot_w.reshape(n_blocks, DISPATCH_BLOCK), blk_expert))
    return acc


def moe_ffn(h, router_w, router_bias, exp_w_gate, exp_w_up, exp_w_down, sh_w_gate, sh_w_up, sh_w_down):
    t_, _ = h.shape
    scores = jax.nn.sigmoid(jnp.matmul(h, router_w, preferred_element_type=jnp.float32))
    sel = scores + router_bias.astype(jnp.float32)
    grp = sel.reshape(t_, N_GROUPS, N_EXPERTS // N_GROUPS)
    grp_score = lax.top_k(grp, 2)[0].sum(-1)
    _, gidx = lax.top_k(grp_score, TOPK_GROUPS)
    gmask = jax.nn.one_hot(gidx, N_GROUPS, dtype=jnp.float32).sum(1) > 0
    emask = jnp.repeat(gmask, N_EXPERTS // N_GROUPS, axis=1)
    _, eidx = lax.top_k(jnp.where(emask, sel, -jnp.inf), TOP_K)
    w = jnp.take_along_axis(scores, eidx, axis=1)
    w = ROUTED_SCALE * w / w.sum(-1, keepdims=True)
    routed = routed_experts(h, eidx, w.astype(h.dtype), exp_w_gate, exp_w_up, exp_w_down)
    return routed + swiglu(h, sh_w_gate, sh_w_up, sh_w_down)


def setup_inputs(seed: int = 0) -> dict:
    key = jax.random.key(seed)
    k = jax.random.split(key, 32)
    L, D = DEPTH, D_MODEL
    f32 = jnp.float32

    def dense(kk, shape, fan_in, gain=1.0):
        return (gain * fan_in ** -0.5) * jax.random.normal(kk, shape, f32)

    def norm_gain(kk, shape):
        return 1.0 + 0.05 * jax.random.normal(kk, shape, f32)

    def small(kk, shape, s=0.02):
        return s * jax.random.normal(kk, shape, f32)

    u = jax.random.uniform(k[19], (L, 2, LRU_WIDTH), f32, 0.9, 0.999)
    a_base = u ** (1.0 / RG_C)
    lru_lambda = jnp.log(a_base) - jnp.log1p(-a_base)
    return {
        "x": jax.random.normal(k[0], (BATCH, SEQ, D), f32),
        "c": jax.random.normal(k[1], (BATCH, D), f32),
        "ctx": jax.random.normal(k[2], (BATCH, CTX_LEN, D), f32),
        "c_ctx": jax.random.normal(k[3], (D,), f32),
        "w_mod": dense(k[4], (L, D, 6 * D), D, 0.5),
        "b_mod": small(k[5], (L, 6 * D)),
        "norm_mix_g": norm_gain(k[6], (L, D)),
        "w_in": dense(k[7], (L, D, IN_COLS), D),
        "q_norm_g": norm_gain(k[8], (L, MLA_Q_RANK)),
        "w_q_up": dense(k[9], (L, MLA_Q_RANK, MLA_HEADS * (MLA_NOPE + MLA_ROPE)), MLA_Q_RANK),
        "kv_norm_g": norm_gain(k[10], (L, MLA_KV_RANK)),
        "w_kv_up": dense(k[11], (L, MLA_KV_RANK, MLA_HEADS * (MLA_NOPE + MLA_V)), MLA_KV_RANK),
        "conv_w": dense(k[12], (L, CONV_W, LRU_WIDTH), CONV_W),
        "conv_b": small(k[13], (L, LRU_WIDTH)),
        "lru_w_a": dense(k[14], (L, 2, LRU_BLOCKS, LRU_BLOCK, LRU_BLOCK), LRU_BLOCK),
        "lru_b_a": small(k[15], (L, 2, LRU_WIDTH)),
        "lru_w_x": dense(k[16], (L, 2, LRU_BLOCKS, LRU_BLOCK, LRU_BLOCK), LRU_BLOCK),
        "lru_b_x": small(k[17], (L, 2, LRU_WIDTH)),
        "lru_lambda": lru_lambda,
        "w_out": dense(k[18], (L, MIX_WIDTH, D), MIX_WIDTH),
        "norm_ffn_g": norm_gain(k[20], (L, D)),
        "router_w": dense(k[21], (L, D, N_EXPERTS), D),
        "router_bias": small(k[22], (L, N_EXPERTS), 0.01),
        "exp_w_gate": dense(k[23], (L, N_EXPERTS, D, EXPERT_FF), D),
        "exp_w_up": dense(k[24], (L, N_EXPERTS, D, EXPERT_FF), D),
        "exp_w_down": dense(k[25], (L, N_EXPERTS, EXPERT_FF, D), EXPERT_FF),
        "sh_w_gate": dense(k[26], (L, D, SHARED_FF), D),
        "sh_w_up": dense(k[27], (L, D, SHARED_FF), D),
        "sh_w_down": dense(k[28], (L, SHARED_FF, D), SHARED_FF),
        "final_norm_g": norm_gain(k[29], (D,)),
    }


def reference(x, c, ctx, c_ctx, w_mod, b_mod, norm_mix_g, w_in, q_norm_g, w_q_up, kv_norm_g, w_kv_up,
              conv_w, conv_b, lru_w_a, lru_b_a, lru_w_x, lru_b_x, lru_lambda, w_out, norm_ffn_g,
              router_w, router_bias, exp_w_gate, exp_w_up, exp_w_down, sh_w_gate, sh_w_up, sh_w_down,
              final_norm_g):
    b_, s_, d_ = x.shape
    n_ctx = ctx.shape[1]
    cos, sin = axial_rope_tables(s_)
    for l in range(DEPTH):
        last = l == DEPTH - 1
        m_lat = jax.nn.silu(c) @ w_mod[l] + b_mod[l]
        m_ctx = jax.nn.silu(c_ctx) @ w_mod[l] + b_mod[l]
        sh1, sc1, g1, sh2, sc2, g2 = jnp.split(m_lat[:, None, :], 6, axis=-1)
        csh1, csc1, cg1, csh2, csc2, cg2 = jnp.split(m_ctx, 6)

        h_lat = modulate(rmsnorm(x, norm_mix_g[l]), sh1, sc1)
        h_ctx = modulate(rmsnorm(ctx, norm_mix_g[l]), csh1, csc1)
        y_lat, y_ctx = hybrid_mixer(h_lat, h_ctx, cos, sin, w_in[l], q_norm_g[l], w_q_up[l], kv_norm_g[l],
                                    w_kv_up[l], conv_w[l], conv_b[l], lru_w_a[l], lru_b_a[l], lru_w_x[l],
                                    lru_b_x[l], lru_lambda[l], not last)
        x = x + g1 * (y_lat @ w_out[l])
        if not last:
            ctx = ctx + cg1 * (y_ctx @ w_out[l])

        f_lat = modulate(rmsnorm(x, norm_ffn_g[l]), sh2, sc2).reshape(-1, d_)
        moe_args = (router_w[l], router_bias[l], exp_w_gate[l], exp_w_up[l], exp_w_down[l],
                    sh_w_gate[l], sh_w_up[l], sh_w_down[l])
        if last:
            x = x + g2 * moe_ffn(f_lat, *moe_args).reshape(b_, s_, d_)
        else:
            f_ctx = modulate(rmsnorm(ctx, norm_ffn_g[l]), csh2, csc2).reshape(-1, d_)
            out = moe_ffn(jnp.concatenate([f_lat, f_ctx], axis=0), *moe_args)
            x = x + g2 * out[:b_ * s_].reshape(b_, s_, d_)
            ctx = ctx + cg2 * out[b_ * s_:].reshape(b_, n_ctx, d_)
    return rmsnorm(x, final_norm_g)
```

```python
import contextlib
import math
import numpy as np
import ml_dtypes
import concourse.bass as bass
import concourse.mybir as mybir
from concourse.bass_utils import run_bass_kernel_spmd

F32 = mybir.dt.float32
BF16 = mybir.dt.bfloat16
I32 = mybir.dt.int32
U32 = mybir.dt.uint32
ALU = mybir.AluOpType
AF = mybir.ActivationFunctionType
AX = mybir.AxisListType

ENGS = ("pe", "act", "dve", "pool", "sp")

D = 1024
S = 4096
NCTX = 256
T = S + NCTX
NE = 64
EPS = 1e-6
ATTN_SCALE = 96 ** -0.5
NBLK = 320
NSLOT = NBLK * 128
NWB6 = 5
RUN6 = NBLK // NWB6
DEBUG = {}


class R:
    __slots__ = ("name", "w", "rs")

    def __init__(self, name=""):
        self.name = name
        self.w = None
        self.rs = []


class K:
    def __init__(self, nc, n_dma_sems=72):
        self.nc = nc
        self.es = contextlib.ExitStack()
        self.ops = {e: [] for e in ENGS}
        self.nops = {e: 0 for e in ENGS}
        self.signal = {e: set() for e in ENGS}
        self.seen = {e: {} for e in ENGS}
        self.esem = {e: self.es.enter_context(nc.semaphore("s_" + e)) for e in ENGS}
        self.dsems = [self.es.enter_context(nc.semaphore("d%d" % i)) for i in range(n_dma_sems)]
        self.dcnt = [0] * n_dma_sems
        self.dlast = [None] * n_dma_sems
        self.dnext = 0
        self.final_tokens = []
        self.all_dma = []
        self.n_hw = n_dma_sems - 24
        self.swnext = self.n_hw
        self.sw_recs = []

    def sbuf(self, name, shape, dtype, stack=None):
        return (stack or self.es).enter_context(self.nc.sbuf_tensor(name, list(shape), dtype))

    def _deps(self, eng, reads, writes):
        toks = []
        for r in reads:
            if r.w is not None:
                toks.append(r.w)
        for r in writes:
            if r.w is not None:
                toks.append(r.w)
            toks.extend(r.rs)
        return self._mk_waits(eng, toks)

    def _resolve_sw(self, swid):
        rec = self.sw_recs[swid]
        if rec["tok"] is None:
            self.ops["pool"].append(("swdone", [], None, rec["si"]))
            idx = self.nops["pool"]
            self.nops["pool"] += 1
            dummy = self.dummy
            self.ops["pool"].append(("op", [], (lambda e: e.memset(dummy[0:1, 0:1], 0.0)), idx))
            rec["tok"] = ("e", "pool", idx)
        return rec["tok"]

    def _mk_waits(self, eng, toks):
        waits = []
        for t in toks:
            if t[0] == "sw":
                t = self._resolve_sw(t[1])
            if t[0] == "e":
                _, x, n = t
                if x == "pe" and eng == "pe":
                    continue
                if self.seen[eng].get(x, -1) >= n:
                    continue
                self.seen[eng][x] = n
                self.signal[x].add(n)
                waits.append(t)
            else:
                _, si, val = t
                key = ("d", si)
                if self.seen[eng].get(key, -1) >= val:
                    continue
                self.seen[eng][key] = val
                waits.append(t)
        return waits

    def _record(self, tok, reads, writes):
        for r in writes:
            r.w = tok
            r.rs = []
        for r in reads:
            if r not in writes:
                r.rs.append(tok)
                if len(r.rs) > 32:
                    best = {}
                    for t in r.rs:
                        k_ = (t[0], t[1])
                        if k_ not in best or best[k_][2] < t[2]:
                            best[k_] = t
                    r.rs = list(best.values())

    def op(self, eng, fn, reads=(), writes=()):
        waits = self._deps(eng, reads, writes)
        idx = self.nops[eng]
        self.nops[eng] += 1
        self.ops[eng].append(("op", waits, fn, idx))
        tok = ("e", eng, idx)
        self._record(tok, reads, writes)
        return tok

    def dma(self, eng, fn, reads=(), writes=(), final=False):
        if eng == "pool":
            lo, hi = self.n_hw, len(self.dsems)
            si = self.swnext
            self.swnext = lo + (si - lo + 1) % (hi - lo)
        else:
            si = self.dnext
            self.dnext = (self.dnext + 1) % self.n_hw
        waits = self._deps(eng, reads, writes)
        if self.dlast[si] is not None:
            key = ("d", si)
            if self.seen[eng].get(key, -1) < self.dlast[si]:
                self.seen[eng][key] = self.dlast[si]
                waits.append(("d", si, self.dlast[si]))
        self.dcnt[si] += 16
        val = self.dcnt[si]
        self.dlast[si] = val
        self.ops[eng].append(("dma", waits, fn, si))
        tok = ("d", si, val)
        self._record(tok, reads, writes)
        if final:
            self.final_tokens.append(tok)
        return tok

    def barrier(self):
        toks = []
        for swid in range(len(self.sw_recs)):
            self._resolve_sw(swid)
        for x in ENGS:
            if self.nops[x] > 0:
                toks.append(("e", x, self.nops[x] - 1))
        for si in range(len(self.dsems)):
            if self.dlast[si] is not None:
                toks.append(("d", si, self.dlast[si]))
        for e in ENGS:
            waits = self._mk_waits(e, toks)
            if waits:
                self.ops[e].append(("wait", waits, None, None))

    def wait_all(self, eng, toks):
        waits = self._mk_waits(eng, toks)
        if waits:
            self.ops[eng].append(("wait", waits, None, None))

    def emit(self):
        nc = self.nc
        pref = {}
        for e in ENGS:
            sig = self.signal[e]
            c = 0
            arr = []
            for i in range(self.nops[e]):
                if i in sig:
                    c += 1
                arr.append(c)
            pref[e] = arr

        def run(e, engobj):
            for kind, waits, fn, extra in self.ops[e]:
                for t in waits:
                    if t[0] == "e":
                        engobj.wait_ge(self.esem[t[1]], pref[t[1]][t[2]])
                    else:
                        engobj.wait_ge(self.dsems[t[1]], t[2])
                if kind == "op":
                    ins = fn(engobj)
                    if extra in self.signal[e]:
                        ins.then_inc(self.esem[e], 1)
                elif kind == "dma":
                    ins = fn(engobj)
                    ins.then_inc(self.dsems[extra], 16)
                elif kind == "swdma":
                    ins = fn(engobj)
                    ins.then_inc(self.swsems[extra], 16)
                elif kind == "swdone":
                    engobj.wait_ge(self.swsems[extra], 16)
                    if getattr(self, "sw_clear", True):
                        engobj.sem_clear(self.swsems[extra])

        with nc.Block() as block:
            @block.tensor
            def _(e):
                run("pe", e)

            @block.scalar
            def _(e):
                run("act", e)

            @block.vector
            def _(e):
                run("dve", e)

            @block.gpsimd
            def _(e):
                run("pool", e)

            @block.sync
            def _(e):
                run("sp", e)

    def MM(self, out, lhsT, rhs, start, stop, reads, writes):
        return self.op("pe", lambda e: e.matmul(out, lhsT, rhs, start=start, stop=stop), reads, writes)

    def TR(self, out, in_, ident, reads, writes):
        return self.op("pe", lambda e: e.transpose(out, in_, ident), reads, writes)

    def ACT(self, out, in_, func, reads, writes, bias=None, scale=None, accum=None):
        kw = {}
        if bias is not None:
            kw["bias"] = bias
        if scale is not None:
            kw["scale"] = scale
        if accum is not None:
            kw["accum_out"] = accum
        return self.op("act", lambda e: e.activation(out=out, in_=in_, func=func, **kw), reads, writes)

    def TS(self, eng, out, in0, s1, s2, op0, op1, reads, writes):
        if op1 is None:
            return self.op(eng, lambda e: e.tensor_scalar(out=out, in0=in0, scalar1=s1, scalar2=None, op0=op0), reads, writes)
        return self.op(eng, lambda e: e.tensor_scalar(out=out, in0=in0, scalar1=s1, scalar2=s2, op0=op0, op1=op1), reads, writes)

    def TT(self, eng, out, in0, in1, op, reads, writes):
        return self.op(eng, lambda e: e.tensor_tensor(out=out, in0=in0, in1=in1, op=op), reads, writes)

    def STT(self, out, in0, scalar, in1, op0, op1, reads, writes):
        return self.op("dve", lambda e: e.scalar_tensor_tensor(out=out, in0=in0, scalar=scalar, in1=in1, op0=op0, op1=op1), reads, writes)

    def CP(self, eng, out, in_, reads, writes):
        if eng == "act":
            return self.ACT(out, in_, AF.Copy, reads, writes)
        return self.op(eng, lambda e: e.tensor_copy(out, in_), reads, writes)

    def MS(self, eng, ap, val, writes):
        return self.op(eng, lambda e: e.memset(ap, val), (), writes)

    def RCP(self, out, in_, reads, writes):
        return self.op("dve", lambda e: e.reciprocal(out=out, in_=in_), reads, writes)

    def DMA(self, eng, out, in_, reads=(), writes=(), final=False):
        return self.dma(eng, lambda e: e.dma_start(out=out, in_=in_), reads, writes, final=final)


def build_nc(debug=None):
    debug = debug or set()
    nc = bass.Bass("TRN2", target_bir_lowering=False)

    def din(name, shape, dt=F32):
        return nc.dram_tensor(name, list(shape), dt, kind="ExternalInput").ap()

    x_d = din("x", [S, D])
    ctx_d = din("ctx", [NCTX, D])
    cT_d = din("cT", [128, 8, 2])
    wmod_d = din("w_mod", [D, 6 * D])
    bmT_d = din("bmT", [128, 48])
    bmrow_d = din("bmrow", [1, 6 * D])
    nmg_d = din("nmgT", [128, 8])
    nfg_d = din("nfgT", [128, 8])
    win_d = din("w_in", [D, 1440])
    qng_d = din("qngT", [128, 2])
    wq_d = din("w_q_up", [256, 768])
    kvng_d = din("kvngT", [128, 1])
    wkv_d = din("w_kv_up", [128, 1024])
    cw_d = din("cwT", [128, 4, 4])
    cb_d = din("cbT", [128, 4])
    lwa_d = din("lru_w_a", [2, 8, 64, 64])
    lwx_d = din("lru_w_x", [2, 8, 64, 64])
    lba_d = din("lbaT", [128, 2, 4])
    lbx_d = din("lbxT", [128, 2, 4])
    lam_d = din("lamT", [128, 2, 4])
    wout_d = din("w_out", [D, D])
    rw_d = din("router_w", [D, NE])
    rb_d = din("rbias_b", [128, NE])
    eg_d = din("exp_w_gate", [NE, D, 256])
    eu_d = din("exp_w_up", [NE, D, 256])
    ed_d = din("exp_w_down", [NE, 256, D])
    sg_d = din("sh_w_gate", [D, 256])
    su_d = din("sh_w_up", [D, 256])
    sd_d = din("sh_w_down", [256, D])
    fng_d = din("fng_b", [128, D])
    cos_d = din("cosT", [32, S])
    sin_d = din("sinT", [32, S])
    tri_d = din("tri_bf", [128, 128], BF16)
    iota_d = din("iota64", [128, NE])
    bstart_d = din("bstart", [128, NBLK])
    pidx_d = din("pidx", [128, 1])
    nfgb_d = din("nfg_b", [128, D])
    idb_d = din("ident_bf", [128, 128], BF16)
    idf_d = din("ident_f", [128, 128])
    y_d = nc.dram_tensor("y", [S, D], F32, kind="ExternalOutput").ap()

    lx_s = nc.dram_tensor("lx_s", [512, T], F32, kind="Internal").ap()
    lg_s = nc.dram_tensor("lg_s", [512, S], F32, kind="Internal").ap()
    x1_s = nc.dram_tensor("x1_s", [S, D], F32, kind="Internal").ap()
    yT_s = nc.dram_tensor("yT_s", [D, S], BF16, kind="Internal").ap()
    W_all = nc.dram_tensor("W_all", [NE * 128, 6144], BF16, kind="Internal").ap()
    ftok_s = nc.dram_tensor("ftok_s", [S, D], BF16, kind="Internal").ap()
    Xs = nc.dram_tensor("Xs", [NSLOT, D], BF16, kind="Internal").ap()
    Ys = nc.dram_tensor("Ys", [NSLOT, D], F32, kind="Internal").ap()
    fT_s = nc.dram_tensor("fT_s", [D, S], BF16, kind="Internal").ap()

    dbg = {}

    k = K(nc)
    PB = [k.es.enter_context(nc.psum_tensor("pb%d" % i, [128, 512], F32)) for i in range(8)]
    RPB = [R("pb%d" % i) for i in range(8)]

    ones_f = k.sbuf("ones_f", [128, 128], F32); r_ones = R()
    k.MS("pool", ones_f[:], 1.0, [r_ones])
    eps_t = k.sbuf("eps_t", [128, 1], F32); r_eps = R()
    k.MS("pool", eps_t[:], EPS, [r_eps])
    one_t = k.sbuf("one_t", [128, 1], F32); r_one = R()
    k.MS("pool", one_t[:], 1.0, [r_one])
    ident_bf = k.sbuf("ident_bf_sb", [128, 128], BF16); r_idb = R()
    k.DMA("sp", ident_bf[:], idb_d, writes=[r_idb])
    ident_f = k.sbuf("ident_f_sb", [128, 128], F32); r_idf = R()
    k.DMA("sp", ident_f[:], idf_d, writes=[r_idf])

    modT = k.sbuf("modT", [128, 48, 2], F32); r_modT = R()
    A1 = k.sbuf("A1", [128, 8, 2], F32); r_A1 = R()
    A2 = k.sbuf("A2", [128, 8], F32); r_A2 = R()
    G1b = k.sbuf("G1b", [128, D], F32); r_G1b = R()
    G2b = k.sbuf("G2b", [128, D], F32); r_G2b = R()
    stat = k.sbuf("stat", [128, 64], F32)
    stat_r = [R() for _ in range(16)]

    A2b = k.sbuf("A2b", [128, D], F32); r_A2b = R()
    B2b = k.sbuf("B2b", [128, D], F32); r_B2b = R()
    rkc = k.sbuf("rkc", [128, 32, NE], F32); r_rkc = R()
    eidx8f = k.sbuf("eidx8f", [128, 32, 8], F32); r_eidx8f = R()
    W8 = k.sbuf("W8", [128, 32, 8], F32); r_W8 = R()
    dest8u = k.sbuf("dest8u", [128, 32, 8], U32); r_dest8u = [R() for _ in range(32)]
    cnt_run = k.sbuf("cnt_run", [128, NE], F32); r_cnt = R()
    idxW = k.sbuf("idxW", [128, NBLK], U32); r_idxW = R()
    tri_bf = k.sbuf("tri_sb", [128, 128], BF16); r_tri = R()
    k.DMA("sp", tri_bf[:], tri_d, writes=[r_tri])
    ones_bf = k.sbuf("ones_bf", [128, 128], BF16); r_onesb = R()
    k.MS("pool", ones_bf[:], 1.0, [r_onesb])
    iota64 = k.sbuf("iota_sb", [128, NE], F32); r_iota = R()
    k.DMA("sp", iota64[:], iota_d, writes=[r_iota])
    zt = k.sbuf("zt", [128, D], BF16); r_zt = R()
    k.MS("pool", zt[:], 0.0, [r_zt])
    r_Wall = R()
    r_ftoks = R()
    r_Xs = R()
    r_Ys = R()
    r_yTs = R()
    r_fTs = R()
    r_x1s = R()
    r_lxs = R()
    r_lgs = R()

    with contextlib.ExitStack() as ph:
        cT = k.sbuf("cT_sb", [128, 8, 2], F32, ph); r_cT = R()
        k.DMA("sp", cT[:], cT_d, writes=[r_cT])
        sc = k.sbuf("sc", [128, 8, 2], F32, ph); r_sc = R()
        k.ACT(sc[:], cT[:], AF.Silu, [r_cT], [r_sc])
        bmT = k.sbuf("bmT_sb", [128, 48], F32, ph); r_bmT = R()
        k.DMA("sp", bmT[:], bmT_d, writes=[r_bmT])
        bmrow = k.sbuf("bmrow_sb", [1, 6 * D], F32, ph); r_bmrow = R()
        k.DMA("sp", bmrow[:], bmrow_d, writes=[r_bmrow])
        nmg = k.sbuf("nmg", [128, 8], F32, ph); r_nmg = R()
        k.DMA("sp", nmg[:], nmg_d, writes=[r_nmg])
        nfg = k.sbuf("nfg", [128, 8], F32, ph); r_nfg = R()
        k.DMA("sp", nfg[:], nfg_d, writes=[r_nfg])
        grow = k.sbuf("grow", [1, 2, D], F32, ph); r_grow = R()
        grow2 = k.sbuf("grow2", [1, 2, D], F32, ph)
        nfgb = k.sbuf("nfgb", [128, D], F32, ph); r_nfgb = R()
        k.DMA("sp", nfgb[:], nfgb_d, writes=[r_nfgb])
        wm = [k.sbuf("wm%d" % i, [128, 8, 512], F32, ph) for i in range(3)]
        r_wm = [R() for _ in range(3)]
        wmod_v = wmod_d.rearrange("(k p) n -> p k n", p=128)
        psM = PB[0]
        for j in range(12):
            b = j % 3
            k.DMA("sp", wm[b][:], wmod_v[:, :, j * 512:(j + 1) * 512], writes=[r_wm[b]])
            v = j // 2
            if v in (2, 3, 4, 5):
                half = j % 2
                pr = PB[1 + (j % 2)]
                rr = RPB[1 + (j % 2)]
                for kk in range(8):
                    k.MM(pr[0:1, :], sc[:, kk, 0:1], wm[b][:, kk, :], kk == 0, kk == 7, [r_sc, r_wm[b]], [rr])
                tgt = {2: grow[0:1, 0, :], 5: grow[0:1, 1, :], 3: grow2[0:1, 0, :], 4: grow2[0:1, 1, :]}[v]
                k.TT("dve", tgt[:, half * 512:(half + 1) * 512], pr[0:1, :],
                     bmrow[0:1, j * 512:(j + 1) * 512], ALU.add, [rr, r_bmrow], [r_grow])
            if v not in (2, 5):
                for cc in range(4):
                    m = j * 4 + cc
                    for kk in range(8):
                        k.MM(psM[:, m * 2:m * 2 + 2], wm[b][:, kk, cc * 128:(cc + 1) * 128], sc[:, kk, :],
                             kk == 0, kk == 7, [r_sc, r_wm[b]], [RPB[0]])
        psM_v = psM[:, 0:96].rearrange("p (m j) -> p m j", j=2)
        k.MS("pool", modT[:], 0.0, [r_modT])
        for jj in range(2):
            for (m0, m1) in ((0, 16), (24, 40)):
                k.TT("dve", modT[:, m0:m1, jj], psM_v[:, m0:m1, jj], bmT[:, m0:m1], ALU.add, [RPB[0], r_bmT], [r_modT])
        for (srow, Gb, rG) in ((grow[0:1, 0, :], G1b, r_G1b), (grow[0:1, 1, :], G2b, r_G2b),
                               (grow2[0:1, 0, :], B2b, r_B2b), (grow2[0:1, 1, :], A2b, r_A2b)):
            for half in range(2):
                pr = PB[3 + half]
                rr = RPB[3 + half]
                k.MM(pr[:, :], ones_f[0:1, :], srow[:, half * 512:(half + 1) * 512], True, True,
                     [r_ones, r_grow], [rr])
                k.CP("act", Gb[:, half * 512:(half + 1) * 512], pr[:, :], [rr], [rG])
        k.TS("dve", A2b[:], A2b[:], 1.0, None, ALU.add, None, [r_A2b], [r_A2b])
        k.TT("dve", A2b[:], A2b[:], nfgb[:], ALU.mult, [r_A2b, r_nfgb], [r_A2b])
        tmpA = k.sbuf("tmpA", [128, 8, 2], F32, ph); r_tmpA = R()
        k.TS("dve", tmpA[:], modT[:, 8:16, :], 1.0, None, ALU.add, None, [r_modT], [r_tmpA])
        for jj in range(2):
            k.TT("dve", A1[:, :, jj], tmpA[:, :, jj], nmg[:], ALU.mult, [r_tmpA, r_nmg], [r_A1])
        tmpB = k.sbuf("tmpB", [128, 8], F32, ph); r_tmpB = R()
        k.TS("dve", tmpB[:], modT[:, 32:40, 0], 1.0, None, ALU.add, None, [r_modT], [r_tmpB])
        k.TT("dve", A2[:], tmpB[:], nfg[:], ALU.mult, [r_tmpB, r_nfg], [r_A2])
        k.barrier()

    mix = contextlib.ExitStack()
    qlatn = k.sbuf("qlatn", [128, 2, S], BF16, mix); r_qlatn = R()
    kvlatn = k.sbuf("kvlatn", [128, T], BF16, mix); r_kvlatn = R()
    krT = k.sbuf("krT", [96, T], BF16, mix); r_krT = R()

    stat_i = [0]

    def rstd_from(ss_ap, r_ss, n):
        i = stat_i[0] % 16
        stat_i[0] += 1
        col = stat[:, i * 4:i * 4 + 4]
        rr = stat_r[i]
        k.ACT(col[:, 1:2], ss_ap, AF.Sqrt, [r_ss, r_eps], [rr], bias=eps_t[:], scale=1.0 / n)
        k.RCP(col[:, 2:3], col[:, 1:2], [rr], [rr])
        return col[:, 2:3], rr

    def new_stat():
        i = stat_i[0] % 16
        return stat[:, i * 4:i * 4 + 1], stat_r[i]

    groups = [(0, NCTX, True)] + [(NCTX + g * 512, 512, False) for g in range(8)]
    with contextlib.ExitStack() as ph:
        win = k.sbuf("win", [128, 8, 1440], BF16, ph); r_win = R()
        win_v = win_d.rearrange("(k p) n -> p k n", p=128)
        for kk in range(8):
            k.DMA("pool", win[:, kk, :], win_v[:, kk, :], writes=[r_win])
        wkr = k.sbuf("wkr", [128, 8, 96], BF16, ph); r_wkr = R()
        wkrs = k.sbuf("wkrs", [128, 8, 96], BF16, ph); r_wkrs = R()
        k.MS("pool", wkr[:], 0.0, [r_wkr])
        k.MS("pool", wkrs[:], 0.0, [r_wkrs])
        k.CP("pool", wkr[:, :, 64:96], win[:, :, 384:416], [r_win], [r_wkr])
        for a in range(2):
            k.TS("pool", wkrs[:, :, 64 + a * 16:64 + a * 16 + 8], win[:, :, 384 + a * 16 + 8:384 + a * 16 + 16],
                 -1.0, None, ALU.mult, None, [r_win], [r_wkrs])
            k.CP("pool", wkrs[:, :, 64 + a * 16 + 8:64 + a * 16 + 16], win[:, :, 384 + a * 16:384 + a * 16 + 8],
                 [r_win], [r_wkrs])
        cs = [k.sbuf("cs%d" % i, [96, 2, 512], F32, ph) for i in range(2)]
        r_cs = [R() for _ in range(2)]

        xt = [k.sbuf("xt%d" % i, [128, D], F32, ph) for i in range(3)]
        r_xt = [R() for _ in range(3)]
        xs = [k.sbuf("xs%d" % i, [128, 4, D], BF16, ph) for i in range(2)]
        r_xs = [R() for _ in range(2)]
        junk = k.sbuf("junk", [128, D], BF16, ph); r_junk = R()
        hT = [k.sbuf("hT%d" % i, [128, 8, 512], BF16, ph) for i in range(2)]
        r_hT = [[R() for _ in range(8)] for _ in range(2)]
        stg = [k.sbuf("stg%d" % i, [128, 512], F32, ph) for i in range(4)]
        r_stg = [R() for _ in range(4)]
        qraw = [k.sbuf("qraw%d" % i, [128, 512], F32, ph) for i in range(3)]
        r_qraw = [R() for _ in range(3)]
        sq = [k.sbuf("sq%d" % i, [128, 512], F32, ph) for i in range(3)]
        r_sq = [R() for _ in range(3)]
        rbc = k.sbuf("rbc", [128, 512], F32, ph); r_rbc = R()
        rt1 = k.sbuf("rt1", [96, 512], F32, ph); r_rt1 = R()
        rt2 = k.sbuf("rt2", [96, 512], F32, ph); r_rt2 = R()
        xti = 0
        stgi = 0
        qri = [0]
        for gi, (t0, n, is_ctx) in enumerate(groups):
            nt = n // 128
            j = 1 if is_ctx else 0
            xb = xs[gi % 2]
            rxb = r_xs[gi % 2]
            hb = hT[gi % 2]
            rhb = r_hT[gi % 2]
            for ti in range(nt):
                b = xti % 3
                xti += 1
                src = ctx_d[ti * 128:(ti + 1) * 128, :] if is_ctx else x_d[t0 - NCTX + ti * 128:t0 - NCTX + (ti + 1) * 128, :]
                k.DMA("sp", xt[b][:], src, writes=[r_xt[b]])
                ss, rss = new_stat()
                k.ACT(junk[:], xt[b][:], AF.Square, [r_xt[b]], [r_junk, rss], accum=ss)
                rstd, rrs = rstd_from(ss, rss, D)
                k.ACT(xb[:, ti, :], xt[b][:], AF.Copy, [r_xt[b], rrs], [rxb], scale=rstd)
            for kk in range(8):
                pb = PB[kk % 2]
                rpb = RPB[kk % 2]
                pbv = pb[:].bitcast(BF16)
                for ti in range(nt):
                    k.TR(pbv[:, ti * 128:(ti + 1) * 128], xb[:, ti, kk * 128:(kk + 1) * 128], ident_bf[:],
                         [rxb, r_idb], [rpb])
                eng = "dve" if kk % 2 == 0 else "pool"
                if eng == "pool":
                    k.ACT(hb[:, kk, 0:n], pbv[:, 0:n], AF.Identity, [rpb, r_A1, r_modT], [rhb[kk]],
                          bias=modT[:, kk, j:j + 1], scale=A1[:, kk, j:j + 1])
                else:
                    k.TS("dve", hb[:, kk, 0:n], pbv[:, 0:n], A1[:, kk, j:j + 1], modT[:, kk, j:j + 1],
                         ALU.mult, ALU.add, [rpb, r_A1, r_modT], [rhb[kk]])
            pbi = [2]

            def nextpb():
                i = pbi[0]
                pbi[0] = 2 + (pbi[0] - 2 + 1) % 5
                return PB[i], RPB[i]

            def proj(cols0, m, lhs_t=None, r_l=None):
                pb, rpb = nextpb()
                for kk in range(8):
                    lhsT = win[:, kk, cols0:cols0 + m] if lhs_t is None else lhs_t[:, kk, 0:m]
                    k.MM(pb[0:m, 0:n], lhsT, hb[:, kk, 0:n], kk == 0, kk == 7,
                         [r_win if r_l is None else r_l, rhb[kk]], [rpb])
                return pb, rpb

            def latent_norm(cols_list, nfeat, dst_fn, r_dst):
                raws = []
                for ci, c0 in enumerate(cols_list):
                    pb, rpb = proj(c0, 128)
                    q = qri[0] % 3
                    qri[0] += 1
                    k.CP("act", qraw[q][:, 0:n], pb[:, 0:n], [rpb], [r_qraw[q]])
                    k.ACT(sq[q][:, 0:n], pb[:, 0:n], AF.Square, [rpb], [r_sq[q]])
                    raws.append(q)
                pbs, rpbs = nextpb()
                for ci, q in enumerate(raws):
                    k.MM(pbs[:, 0:n], ones_f[:], sq[q][:, 0:n], ci == 0, ci == len(raws) - 1, [r_ones, r_sq[q]], [rpbs])
                k.ACT(rbc[:, 0:n], pbs[:, 0:n], AF.Sqrt, [rpbs, r_eps], [r_rbc], bias=eps_t[:], scale=1.0 / nfeat)
                k.RCP(rbc[:, 0:n], rbc[:, 0:n], [r_rbc], [r_rbc])
                for ci, q in enumerate(raws):
                    k.TT("dve", dst_fn(ci), qraw[q][:, 0:n], rbc[:, 0:n], ALU.mult, [r_qraw[q], r_rbc], [r_dst])

            if not is_ctx:
                q0 = t0 - NCTX
                latent_norm([0, 128], 256, lambda ci: qlatn[:, ci, q0:q0 + n], r_qlatn)
            latent_norm([256], 128, lambda ci: kvlatn[:, t0:t0 + n], r_kvlatn)
            pb1, rpb1 = proj(0, 96, wkr, r_wkr)
            if is_ctx:
                k.CP("act", krT[64:96, t0:t0 + n], pb1[64:96, 0:n], [rpb1], [r_krT])
            else:
                pb2, rpb2 = proj(0, 96, wkrs, r_wkrs)
                q0 = t0 - NCTX
                csb, rcsb = cs[gi % 2], r_cs[gi % 2]
                k.DMA("sp", csb[64:96, 0, :], cos_d[:, q0:q0 + n], writes=[rcsb])
                k.DMA("sp", csb[64:96, 1, :], sin_d[:, q0:q0 + n], writes=[rcsb])
                k.TT("dve", rt1[64:96, 0:n], pb1[64:96, 0:n], csb[64:96, 0, 0:n], ALU.mult, [rpb1, rcsb], [r_rt1])
                k.TT("dve", rt2[64:96, 0:n], pb2[64:96, 0:n], csb[64:96, 1, 0:n], ALU.mult, [rpb2, rcsb], [r_rt2])
                k.TT("pool", krT[64:96, t0:t0 + n], rt1[64:96, 0:n], rt2[64:96, 0:n], ALU.add, [r_rt1, r_rt2], [r_krT])
            for c in range(4):
                pb, rpb = proj(416 + c * 128, 128)
                sb = stgi % 4
                stgi += 1
                k.CP("act", stg[sb][:, 0:n], pb[:, 0:n], [rpb], [r_stg[sb]])
                k.DMA("pool", lx_s[c * 128:(c + 1) * 128, t0:t0 + n], stg[sb][:, 0:n], reads=[r_stg[sb]])
            if not is_ctx:
                q0 = t0 - NCTX
                for c in range(4):
                    pb, rpb = proj(928 + c * 128, 128)
                    sb = stgi % 4
                    stgi += 1
                    k.ACT(stg[sb][:, 0:n], pb[:, 0:n], AF.Gelu_apprx_tanh, [rpb], [r_stg[sb]])
                    k.DMA("pool", lg_s[c * 128:(c + 1) * 128, q0:q0 + n], stg[sb][:, 0:n], reads=[r_stg[sb]])
        k.barrier()

    if "p1" in debug:
        dbg["qlatn"] = (qlatn, [128, 2, S], BF16, [r_qlatn])
        dbg["kvlatn"] = (kvlatn, [128, T], BF16, [r_kvlatn])
        dbg["krT"] = (krT, [96, T], BF16, [r_krT])

    with contextlib.ExitStack() as ph:
        wbd = k.sbuf("wbd", [128, 2, 2, 4, 128], BF16, ph); r_wbd = R()
        k.MS("pool", wbd[:], 0.0, [r_wbd])
        for d in range(2):
            for gsel, wsrc in enumerate((lwa_d, lwx_d)):
                for c in range(4):
                    for hb_ in range(2):
                        k.DMA("pool", wbd[hb_ * 64:(hb_ + 1) * 64, d, gsel, c, hb_ * 64:(hb_ + 1) * 64],
                              wsrc[d, 2 * c + hb_, :, :], writes=[r_wbd])
        cw = k.sbuf("cw", [128, 4, 4], F32, ph); r_cw = R()
        k.DMA("sp", cw[:], cw_d, writes=[r_cw])
        cb = k.sbuf("cb", [128, 4], F32, ph)
        k.DMA("sp", cb[:], cb_d, writes=[r_cw])
        lba = k.sbuf("lba", [128, 2, 4], F32, ph)
        lbx = k.sbuf("lbx", [128, 2, 4], F32, ph)
        lam = k.sbuf("lam", [128, 2, 4], F32, ph); r_lam = R()
        k.DMA("sp", lba[:], lba_d, writes=[r_cw])
        k.DMA("sp", lbx[:], lbx_d, writes=[r_cw])
        nlba = k.sbuf("nlba", [128, 2, 4], F32, ph)
        nlbx = k.sbuf("nlbx", [128, 2, 4], F32, ph)
        k.TS("dve", nlba[:], lba[:], -1.0, None, ALU.mult, None, [r_cw], [r_cw])
        k.TS("dve", nlbx[:], lbx[:], -1.0, None, ALU.mult, None, [r_cw], [r_cw])
        k.DMA("sp", lam[:], lam_d, writes=[r_lam])
        cneg = k.sbuf("cneg", [128, 2, 4], F32, ph); r_cneg = R()
        k.ACT(cneg[:], lam[:], AF.Exp, [r_lam], [r_cneg], scale=-1.0)
        k.ACT(cneg[:], cneg[:], AF.Ln, [r_cneg, r_one], [r_cneg], bias=one_t[:], scale=1.0)
        k.TS("dve", cneg[:], cneg[:], -8.0, None, ALU.mult, None, [r_cneg], [r_cneg])

        xc = k.sbuf("xc", [128, T], F32, ph); r_xc = R()
        u = k.sbuf("u", [128, T], F32, ph); r_u = R()
        ub = k.sbuf("ub", [128, T], BF16, ph); r_ub = R()
        hsum = k.sbuf("hsum", [128, S], F32, ph); r_hsum = R()
        gc = k.sbuf("gc", [128, S], F32, ph); r_gc = R()
        NB = 2
        rg = [k.sbuf("rg%d" % i, [128, 512], F32, ph) for i in range(NB)]; r_rg = [R() for _ in range(NB)]
        ig = [k.sbuf("ig%d" % i, [128, 512], F32, ph) for i in range(NB)]; r_ig = [R() for _ in range(NB)]
        a2 = [k.sbuf("a2%d" % i, [128, 512], F32, ph) for i in range(NB)]; r_a2 = [R() for _ in range(NB)]
        hg = [k.sbuf("hg%d" % i, [128, 512], F32, ph) for i in range(NB)]; r_hg = [R() for _ in range(NB)]
        tsum = k.sbuf("tsum", [128, 512], F32, ph); r_tsum = R()
        yst = [k.sbuf("yst%d" % i, [128, 512], BF16, ph) for i in range(2)]; r_yst = [R() for _ in range(2)]
        ysi = 0
        segs = [(0, NCTX), (NCTX, T)]
        it = 0
        for c in range(4):
            k.DMA("sp", xc[:], lx_s[c * 128:(c + 1) * 128, :], reads=[r_lxs], writes=[r_xc])
            k.DMA("sp", gc[:], lg_s[c * 128:(c + 1) * 128, :], reads=[r_lgs], writes=[r_gc])
            for zb in range(c * (NBLK // 4), (c + 1) * (NBLK // 4)):
                k.DMA("sp", Xs[zb * 128:(zb + 1) * 128, :], zt[:], reads=[r_zt])
            for (s0, e0) in segs:
                k.TS("dve", u[:, s0:e0], xc[:, s0:e0], cw[:, c, 2:3], cb[:, c:c + 1], ALU.mult, ALU.add,
                     [r_xc, r_cw], [r_u])
                k.STT(u[:, s0 + 2:e0], xc[:, s0:e0 - 2], cw[:, c, 0:1], u[:, s0 + 2:e0], ALU.mult, ALU.add,
                      [r_xc, r_cw, r_u], [r_u])
                k.STT(u[:, s0 + 1:e0], xc[:, s0:e0 - 1], cw[:, c, 1:2], u[:, s0 + 1:e0], ALU.mult, ALU.add,
                      [r_xc, r_cw, r_u], [r_u])
                k.STT(u[:, s0:e0 - 1], xc[:, s0 + 1:e0], cw[:, c, 3:4], u[:, s0:e0 - 1], ALU.mult, ALU.add,
                      [r_xc, r_cw, r_u], [r_u])
            k.CP("pool", ub[:], u[:], [r_u], [r_ub])
            for d in range(2):
                order = [groups[0]] + (groups[1:] if d == 0 else groups[1:][::-1])
                prev_init = [None]

                def st1(grp, b):
                    (t0, n, is_ctx) = grp
                    pa, rpa = PB[(b * 2) % 8 + (4 if d else 0)], RPB[(b * 2) % 8 + (4 if d else 0)]
                    px, rpx = PB[(b * 2 + 1) % 8 + (4 if d else 0)], RPB[(b * 2 + 1) % 8 + (4 if d else 0)]
                    k.MM(pa[:, 0:n], wbd[:, d, 0, c, :], ub[:, t0:t0 + n], True, True, [r_wbd, r_ub], [rpa])
                    k.MM(px[:, 0:n], wbd[:, d, 1, c, :], ub[:, t0:t0 + n], True, True, [r_wbd, r_ub], [rpx])
                    k.ACT(rg[b][:, 0:n], pa[:, 0:n], AF.Sigmoid, [rpa, r_cw], [r_rg[b]], bias=lba[:, d, c:c + 1])
                    k.ACT(ig[b][:, 0:n], px[:, 0:n], AF.Sigmoid, [rpx, r_cw], [r_ig[b]], bias=lbx[:, d, c:c + 1])

                def st2(grp, b):
                    (t0, n, is_ctx) = grp
                    k.ACT(rg[b][:, 0:n], rg[b][:, 0:n], AF.Exp, [r_rg[b], r_cneg], [r_rg[b]], scale=cneg[:, d, c:c + 1])
                    k.TT("pool", a2[b][:, 0:n], rg[b][:, 0:n], rg[b][:, 0:n], ALU.mult, [r_rg[b]], [r_a2[b]])
                    k.TT("dve", ig[b][:, 0:n], ig[b][:, 0:n], u[:, t0:t0 + n], ALU.mult, [r_ig[b], r_u], [r_ig[b]])

                def st3(grp, b):
                    (t0, n, is_ctx) = grp
                    k.ACT(a2[b][:, 0:n], a2[b][:, 0:n], AF.Sqrt, [r_a2[b], r_one], [r_a2[b]], bias=one_t[:], scale=-1.0)
                    k.TT("dve", a2[b][:, 0:n], a2[b][:, 0:n], ig[b][:, 0:n], ALU.mult, [r_a2[b], r_ig[b]], [r_a2[b]])

                def st4(grp, b):
                    nonlocal ysi
                    (t0, n, is_ctx) = grp
                    if d == 0:
                        o_ap, a_ap, b_ap = hg[b][:, 0:n], rg[b][:, 0:n], a2[b][:, 0:n]
                    else:
                        o_ap, a_ap, b_ap = hg[b][:, 0:n][:, ::-1], rg[b][:, 0:n][:, ::-1], a2[b][:, 0:n][:, ::-1]
                    rds = [r_rg[b], r_a2[b]]
                    if prev_init[0] is None:
                        init = 0.0
                    else:
                        init = prev_init[0][0]
                        rds.append(prev_init[0][1])
                    k.op("dve", (lambda o_ap=o_ap, a_ap=a_ap, b_ap=b_ap, init=init:
                                 (lambda e: e.tensor_tensor_scan(out=o_ap, data0=a_ap, data1=b_ap, initial=init,
                                                                 op0=ALU.mult, op1=ALU.add)))(),
                         rds, [r_hg[b]])
                    last = hg[b][:, n - 1:n] if d == 0 else hg[b][:, 0:1]
                    prev_init[0] = (last, r_hg[b])
                    if not is_ctx:
                        q0 = t0 - NCTX
                        if d == 0:
                            k.CP("pool", hsum[:, q0:q0 + n], hg[b][:, 0:n], [r_hg[b]], [r_hsum])
                        else:
                            k.TT("dve", tsum[:, 0:n], hg[b][:, 0:n], hsum[:, q0:q0 + n], ALU.add, [r_hg[b], r_hsum], [r_tsum])
                            yb, ryb = yst[ysi % 2], r_yst[ysi % 2]
                            ysi += 1
                            k.TT("pool", yb[:, 0:n], tsum[:, 0:n], gc[:, q0:q0 + n], ALU.mult,
                                 [r_tsum, r_gc], [ryb])
                            k.DMA("sp", yT_s[(4 + c) * 128:(5 + c) * 128, q0:q0 + n], yb[:, 0:n], reads=[ryb])

                for p0 in range(0, len(order), NB):
                    pair = [(order[p0 + q], q) for q in range(NB) if p0 + q < len(order)]
                    for (grp, b) in pair:
                        st1(grp, b)
                    for (grp, b) in pair:
                        st2(grp, b)
                    for (grp, b) in pair:
                        st3(grp, b)
                    for (grp, b) in pair:
                        st4(grp, b)
        k.barrier()

    with contextlib.ExitStack() as ph:
        wq = k.sbuf("wq", [128, 2, 768], BF16, ph); r_wq = R()
        wqs = k.sbuf("wqs", [128, 2, 768], BF16, ph); r_wqs = R()
        wkv = k.sbuf("wkv", [128, 1024], BF16, ph); r_wkv = R()
        with contextlib.ExitStack() as ph2:
            wtmp = k.sbuf("wtmp", [128, 2, 1024], F32, ph2); r_wtmp = R()
            qng = k.sbuf("qng", [128, 2], F32, ph2); r_qng = R()
            kvng = k.sbuf("kvng", [128, 1], F32, ph2)
            k.DMA("sp", qng[:], qng_d, writes=[r_qng])
            k.DMA("sp", kvng[:], kvng_d, writes=[r_qng])
            wq_v = wq_d.rearrange("(k p) n -> p k n", p=128)
            k.DMA("sp", wtmp[:, :, 0:768], wq_v, writes=[r_wtmp])
            for m in range(2):
                k.TS("dve", wq[:, m, :], wtmp[:, m, 0:768], qng[:, m:m + 1], None, ALU.mult, None, [r_wtmp, r_qng], [r_wq])
            k.MS("pool", wqs[:], 0.0, [r_wqs])
            for h in range(8):
                for a in range(2):
                    base = h * 96 + 64 + a * 16
                    k.TS("pool", wqs[:, :, base:base + 8], wq[:, :, base + 8:base + 16], -1.0, None, ALU.mult, None,
                         [r_wq], [r_wqs])
                    k.CP("pool", wqs[:, :, base + 8:base + 16], wq[:, :, base:base + 8], [r_wq], [r_wqs])
            wtmp2 = k.sbuf("wtmp2", [128, 1024], F32, ph2); r_wtmp2 = R()
            k.DMA("sp", wtmp2[:], wkv_d, writes=[r_wtmp2])
            k.TS("dve", wkv[:], wtmp2[:], kvng[:, 0:1], None, ALU.mult, None, [r_wtmp2, r_qng], [r_wkv])
            k.barrier()
        cs = [k.sbuf("cs3%d" % i, [96, 2, 512], F32, ph) for i in range(2)]
        r_cs = [R() for _ in range(2)]
        csi = [0]
        yst = [k.sbuf("yst3%d" % i, [64, 512], BF16, ph) for i in range(2)]; r_yst = [R() for _ in range(2)]
        wst = [k.sbuf("wst%d" % i, [128, 6144], BF16, ph) for i in range(2)]; r_wst = [R() for _ in range(2)]

        def precast(e):
            b_ = e % 2
            gu_v = wst[b_][:, 0:4096].rearrange("p (k t f) -> p k t f", t=2, f=256)
            k.DMA("pool", gu_v[:, :, 0, :], eg_d[e].rearrange("(k p) f -> p k f", p=128), writes=[r_wst[b_]])
            k.DMA("pool", gu_v[:, :, 1, :], eu_d[e].rearrange("(k p) f -> p k f", p=128), writes=[r_wst[b_]])
            k.DMA("pool", wst[b_][:, 4096:6144].rearrange("p (k f) -> p k f", f=1024),
                  ed_d[e].rearrange("(k p) f -> p k f", p=128), writes=[r_wst[b_]])
            k.DMA("sp", W_all[e * 128:(e + 1) * 128, :], wst[b_][:], reads=[r_wst[b_]])

        Qh = [k.sbuf("Qh%d" % i, [96, S], BF16, ph) for i in range(2)]; r_Qh = [R() for _ in range(2)]
        Kh = [k.sbuf("Kh%d" % i, [96, T], BF16, ph) for i in range(2)]; r_Kh = [R() for _ in range(2)]
        Va = [k.sbuf("Va%d" % i, [128, 34, 128], BF16, ph) for i in range(2)]; r_Va = [R() for _ in range(2)]
        for i in range(2):
            k.MS("pool", Va[i][:, :, 64:128], 1.0, [r_Va[i]])
        rt1 = k.sbuf("rt1b", [96, 512], F32, ph); r_rt1 = R()
        rt2 = k.sbuf("rt2b", [96, 512], F32, ph); r_rt2 = R()
        PT = [k.sbuf("PT%d" % i, [128, 512], BF16, ph) for i in range(3)]; r_PT = [R() for _ in range(3)]
        rcp = [k.sbuf("rcp%d" % i, [128, 512], F32, ph) for i in range(2)]; r_rcp = [R() for _ in range(2)]
        misc = [5, 6, 7]
        mi = [0]

        def mpb():
            i = misc[mi[0] % 3]
            mi[0] += 1
            return PB[i], RPB[i]

        def build_head(h):
            s = h % 2
            for g in range(8):
                c0 = g * 512
                p1, rp1 = mpb()
                p2, rp2 = mpb()
                for m in range(2):
                    k.MM(p1[0:96, :], wq[:, m, h * 96:(h + 1) * 96], qlatn[:, m, c0:c0 + 512], m == 0, m == 1,
                         [r_wq, r_qlatn], [rp1])
                for m in range(2):
                    k.MM(p2[0:96, :], wqs[:, m, h * 96:(h + 1) * 96], qlatn[:, m, c0:c0 + 512], m == 0, m == 1,
                         [r_wqs, r_qlatn], [rp2])
                k.CP("dve", Qh[s][0:64, c0:c0 + 512], p1[0:64, :], [rp1], [r_Qh[s]])
                csb, rcsb = cs[csi[0] % 2], r_cs[csi[0] % 2]
                csi[0] += 1
                k.DMA("sp", csb[64:96, 0, :], cos_d[:, c0:c0 + 512], writes=[rcsb])
                k.DMA("sp", csb[64:96, 1, :], sin_d[:, c0:c0 + 512], writes=[rcsb])
                k.TT("dve", rt1[64:96, :], p1[64:96, :], csb[64:96, 0, :], ALU.mult, [rp1, rcsb], [r_rt1])
                k.TT("dve", rt2[64:96, :], p2[64:96, :], csb[64:96, 1, :], ALU.mult, [rp2, rcsb], [r_rt2])
                k.TT("pool", Qh[s][64:96, c0:c0 + 512], rt1[64:96, :], rt2[64:96, :], ALU.add, [r_rt1, r_rt2], [r_Qh[s]])
            for (t0, n, _) in groups:
                p1, rp1 = mpb()
                k.MM(p1[0:64, 0:n], wkv[:, h * 128:h * 128 + 64], kvlatn[:, t0:t0 + n], True, True,
                     [r_wkv, r_kvlatn], [rp1])
                k.CP("dve", Kh[s][0:64, t0:t0 + n], p1[0:64, 0:n], [rp1], [r_Kh[s]])
            k.CP("pool", Kh[s][64:96, :], krT[64:96, :], [r_krT], [r_Kh[s]])
            for kt0 in range(0, 34, 8):
                nk = min(8, 34 - kt0)
                p1, rp1 = mpb()
                for i in range(nk):
                    kt = kt0 + i
                    k.MM(p1[:, i * 64:(i + 1) * 64], kvlatn[:, kt * 128:(kt + 1) * 128],
                         wkv[:, h * 128 + 64:h * 128 + 128], True, True, [r_wkv, r_kvlatn], [rp1])
                k.CP("dve", Va[s][:, kt0:kt0 + nk, 0:64], p1[:, 0:nk * 64].rearrange("p (a b) -> p a b", b=64),
                     [rp1], [r_Va[s]])

        def attend(h):
            s = h % 2
            seq = [(qg, kt) for qg in range(8) for kt in range(34)]
            LA = 2

            def issue_S(i):
                qg, kt = seq[i]
                c0 = qg * 512
                if kt == 0:
                    precast(h * 8 + qg)
                ps_, rps = PB[i % 3], RPB[i % 3]
                k.MM(ps_[:, :], Kh[s][:, kt * 128:(kt + 1) * 128], Qh[s][:, c0:c0 + 512], True, True,
                     [r_Kh[s], r_Qh[s]], [rps])

            def issue_rest(i):
                qg, kt = seq[i]
                c0 = qg * 512
                ps_, rps = PB[i % 3], RPB[i % 3]
                po, rpo = PB[3 + (qg % 2)], RPB[3 + (qg % 2)]
                pt, rpt = PT[i % 3], r_PT[i % 3]
                k.ACT(pt[:], ps_[:, :], AF.Exp, [rps], [rpt], scale=ATTN_SCALE)
                k.MM(po[:, :], Va[s][:, kt, :], pt[:], kt == 0, kt == 33, [r_Va[s], rpt], [rpo])
                if kt == 33:
                    rc, rrc = rcp[qg % 2], r_rcp[qg % 2]
                    k.RCP(rc[64:128, :], po[64:128, :], [rpo], [rrc])
                    yb, ryb = yst[qg % 2], r_yst[qg % 2]
                    k.TT("dve", yb[0:64, :], po[0:64, :], rc[64:128, :], ALU.mult, [rpo, rrc], [ryb])
                    k.DMA("sp", yT_s[h * 64:(h + 1) * 64, c0:c0 + 512], yb[0:64, :], reads=[ryb])

            for i in range(len(seq) + LA):
                if i < len(seq):
                    issue_S(i)
                if i >= LA:
                    issue_rest(i - LA)

        build_head(0)
        for h in range(8):
            if h + 1 < 8:
                build_head(h + 1)
            attend(h)
        k.barrier()

    mix.close()

    with contextlib.ExitStack() as ph:
        wout = k.sbuf("wout", [128, 8, D], BF16, ph); r_wout = R()
        wout_v = wout_d.rearrange("(k p) n -> p k n", p=128)
        for kk in range(8):
            k.DMA("pool", wout[:, kk, :], wout_v[:, kk, :], writes=[r_wout])
        wsh = k.sbuf("wsh", [128, 6144], BF16, ph); r_wsh = R()
        k.DMA("pool", wsh[:, 0:2048].rearrange("p (k f) -> p k f", f=256), sg_d.rearrange("(k p) f -> p k f", p=128), writes=[r_wsh])
        k.DMA("pool", wsh[:, 2048:4096].rearrange("p (k f) -> p k f", f=256), su_d.rearrange("(k p) f -> p k f", p=128), writes=[r_wsh])
        k.DMA("pool", wsh[:, 4096:6144].rearrange("p (k f) -> p k f", f=1024), sd_d.rearrange("(k p) f -> p k f", p=128), writes=[r_wsh])
        rw = k.sbuf("rw", [128, 8, NE], F32, ph); r_rw = R()
        k.DMA("sp", rw[:], rw_d.rearrange("(k p) n -> p k n", p=128), writes=[r_rw])
        rbias = k.sbuf("rbias", [128, NE], F32, ph); r_rbias = R()
        k.DMA("sp", rbias[:], rb_d, writes=[r_rbias])
        xt = [k.sbuf("xt4%d" % i, [128, D], F32, ph) for i in range(4)]; r_xt = [R() for _ in range(4)]
        GT = 2
        GN = GT * 128
        x1_l = [k.sbuf("x1b%d" % i, [128, GT, D], F32, ph) for i in range(4)]; r_x1_l = [[R() for _ in range(GT)] for _ in range(4)]
        xs2_l = [k.sbuf("xs2b%d" % i, [128, GT, D], F32, ph) for i in range(2)]; r_xs2_l = [[R() for _ in range(GT)] for _ in range(2)]
        fT32_l = [k.sbuf("fT32b%d" % i, [128, 8, GN], F32, ph) for i in range(2)]; r_fT32_l = [[R() for _ in range(8)] for _ in range(2)]
        yTg_l = [k.sbuf("yTg%d" % i, [128, 8, GN], BF16, ph) for i in range(2)]; r_yTg_l = [R() for _ in range(2)]
        fTb_l = [k.sbuf("fTb%d" % i, [128, 8, GN], BF16, ph) for i in range(2)]; r_fTb_l = [[R() for _ in range(8)] for _ in range(2)]
        tmpf = k.sbuf("tmpf", [128, D], F32, ph); r_tmpf = R(); r_tmpf_h = [R(), R()]
        tmpf2 = k.sbuf("tmpf2", [128, D], F32, ph); r_tmpf2 = R()
        ftk = [k.sbuf("ftk%d" % i, [128, D], BF16, ph) for i in range(2)]; r_ftk = [R() for _ in range(2)]
        sgt = [k.sbuf("sgt4%d" % i, [128, GN], F32, ph) for i in range(2)]; r_sgt = [R() for _ in range(2)]
        hsh_l = [k.sbuf("hsh%d" % i, [128, 2, GN], BF16, ph) for i in range(2)]; r_hsh_l = [R() for _ in range(2)]
        junk4 = k.sbuf("junk4", [128, D], BF16, ph); r_junk4 = R()
        yT_v = yT_s.rearrange("(k p) s -> p k s", p=128)
        scr = k.sbuf("scr", [128, 2 * NE], F32, ph); r_scr = R()
        sel = k.sbuf("sel", [128, 2 * NE], F32, ph); r_sel = R()
        selm = k.sbuf("selm", [128, 2 * NE], F32, ph); r_selm = R()
        eq = k.sbuf("eq", [128, 2 * NE], F32, ph); r_eq = R()
        Mb = k.sbuf("Mb", [128, 2 * NE], BF16, ph); r_Mb = R()
        Wgt = k.sbuf("Wgt", [128, 2 * NE], F32, ph); r_Wgt = R()
        oh = k.sbuf("oh", [128, 8 * NE], F32, ph); r_oh = R()
        g8 = k.sbuf("g8", [128, 3, 16], F32, ph); r_g8 = R()
        t8 = k.sbuf("t8", [128, 16], F32, ph); r_t8 = R()
        i8u = k.sbuf("i8u", [128, 16], U32, ph); r_i8u = R()
        wsum = k.sbuf("wsum", [128, 8], F32, ph); r_wsum = R()
        k.MS("pool", cnt_run[:], 0.0, [r_cnt])
        k.MS("pool", W8[:], 0.0, [r_W8])
        xti_c = [0]

        def bufs(g):
            return (x1_l[g % 4], r_x1_l[g % 4], xs2_l[g % 2], r_xs2_l[g % 2], fT32_l[g % 2], r_fT32_l[g % 2],
                    yTg_l[g % 2], r_yTg_l[g % 2], fTb_l[g % 2], r_fTb_l[g % 2])

        NG4 = S // GN

        def loads4(g):
            if g >= S // GN:
                return
            k.DMA("sp", yTg_l[g % 2][:], yT_v[:, :, g * GN:(g + 1) * GN], reads=[r_yTs], writes=[r_yTg_l[g % 2]])
            for ti in range(GT):
                tok0 = g * GN + ti * 128
                b = (g * GT + ti) % 4
                k.DMA("sp", xt[b][:], x_d[tok0:tok0 + 128, :], writes=[r_xt[b]])

        loads4(0)

        def stageA(g):
            x1, r_x1, xs2, r_xs2, fT32, r_fT32, yTg, r_yTg, fTb, r_fTb = bufs(g)
            loads4(g + 1)
            for ti in range(GT):
                tok0 = g * GN + ti * 128
                b = (g * GT + ti) % 4
                rx1 = r_x1[ti]
                for nn in range(2):
                    po, rpo = PB[(ti * 2 + nn) % 4], RPB[(ti * 2 + nn) % 4]
                    for kk in range(8):
                        k.MM(po[:, :], yTg[:, kk, ti * 128:(ti + 1) * 128], wout[:, kk, nn * 512:(nn + 1) * 512], kk == 0, kk == 7,
                             [r_yTg, r_wout], [rpo])
                    k.TT("dve", x1[:, ti, nn * 512:(nn + 1) * 512], po[:, :], G1b[:, nn * 512:(nn + 1) * 512], ALU.mult,
                         [rpo, r_G1b], [rx1])
                k.TT("dve", x1[:, ti, :], x1[:, ti, :], xt[b][:], ALU.add, [rx1, r_xt[b]], [rx1])
                ss, rss = new_stat()
                k.ACT(junk4[:], x1[:, ti, :], AF.Square, [rx1], [r_junk4, rss], accum=ss)
                rstd, rrs = rstd_from(ss, rss, D)
                k.ACT(xs2[:, ti, :], x1[:, ti, :], AF.Copy, [rx1, rrs], [r_xs2[ti]], scale=rstd)
                fb_, rfb_ = ftk[ti % 2], r_ftk[ti % 2]
                k.TT("dve", tmpf2[:], xs2[:, ti, :], A2b[:], ALU.mult, [r_xs2[ti], r_A2b], [r_tmpf2])
                k.TT("dve", fb_[:], tmpf2[:], B2b[:], ALU.add, [r_tmpf2, r_B2b], [rfb_])
                k.DMA("sp", ftok_s[tok0:tok0 + 128, :], fb_[:], reads=[rfb_])

        def stageA2(g):
            x1, r_x1, xs2, r_xs2, fT32, r_fT32, yTg, r_yTg, fTb, r_fTb = bufs(g)
            for kk in range(8):
                pb, rpb = PB[4 + kk % 2], RPB[4 + kk % 2]
                for ti in range(GT):
                    k.TR(pb[:, ti * 128:(ti + 1) * 128], xs2[:, ti, kk * 128:(kk + 1) * 128], ident_f[:],
                         [r_xs2[ti], r_idf], [rpb])
                if kk % 2 == 0:
                    k.TS("dve", fT32[:, kk, :], pb[:, 0:GN], A2[:, kk:kk + 1], modT[:, 24 + kk, 0:1], ALU.mult, ALU.add,
                         [rpb, r_A2, r_modT], [r_fT32[kk]])
                else:
                    k.ACT(fT32[:, kk, :], pb[:, 0:GN], AF.Identity, [rpb, r_A2, r_modT], [r_fT32[kk]],
                          bias=modT[:, 24 + kk, 0:1], scale=A2[:, kk:kk + 1])
                k.CP("pool", fTb[:, kk, :], fT32[:, kk, :], [r_fT32[kk]], [r_fTb[kk]])
            pr, rpr = PB[6], RPB[6]
            pk, rpk = PB[7], RPB[7]
            for ti in range(GT):
                for kk in range(8):
                    k.MM(pr[:, ti * NE:(ti + 1) * NE], fT32[:, kk, ti * 128:(ti + 1) * 128], rw[:, kk, :], kk == 0, kk == 7,
                         [r_fT32[kk], r_rw], [rpr])
            k.ACT(scr[:], pr[:, 0:GT * NE], AF.Sigmoid, [rpr], [r_scr])
            scr3 = scr[:].rearrange("p (t e) -> p t e", e=NE)
            sel3t = sel[:].rearrange("p (t e) -> p t e", e=NE)
            k.TT("dve", sel3t, scr3, rbias[:].unsqueeze(1).to_broadcast([128, GT, NE]), ALU.add, [r_scr, r_rbias], [r_sel])
            selg = sel[:].rearrange("p (a b) -> p a b", b=8)
            eqg = eq[:].rearrange("p (a b) -> p a b", b=8)
            selmg = selm[:].rearrange("p (a b) -> p a b", b=8)
            k.op("dve", (lambda o=g8[:, 0, :], i=selg: (lambda e: e.tensor_reduce(out=o, in_=i, axis=AX.X, op=ALU.max)))(),
                 [r_sel], [r_g8])
            k.TT("dve", eqg, selg, g8[:, 0, :].unsqueeze(2).to_broadcast([128, GT * 8, 8]), ALU.is_equal, [r_sel, r_g8], [r_eq])
            k.STT(selm[:], eq[:], -1e9, sel[:], ALU.mult, ALU.add, [r_eq, r_sel], [r_selm])
            k.op("dve", (lambda o=g8[:, 1, :], i=selmg: (lambda e: e.tensor_reduce(out=o, in_=i, axis=AX.X, op=ALU.max)))(),
                 [r_selm], [r_g8])
            k.TT("dve", g8[:, 1, :], g8[:, 1, :], g8[:, 0, :], ALU.add, [r_g8], [r_g8])
            gs3 = g8[:, 1, :].rearrange("p (t b) -> p t b", b=8)
            t8v = t8[:].rearrange("p (t b) -> p t b", b=8)
            for ti in range(GT):
                k.op("dve", (lambda o=t8[:, ti * 8:(ti + 1) * 8], i=g8[:, 1, ti * 8:(ti + 1) * 8]: (lambda e: e.max(out=o, in_=i)))(),
                     [r_g8], [r_t8])
            pen3 = g8[:, 2, :].rearrange("p (t b) -> p t b", b=8)
            k.TT("dve", pen3, gs3, t8v[:, :, 3:4].to_broadcast([128, GT, 8]), ALU.is_ge, [r_g8, r_t8], [r_g8])
            k.TS("dve", g8[:, 2, :], g8[:, 2, :], 1.0, 1e9, ALU.subtract, ALU.mult, [r_g8], [r_g8])
            k.TT("dve", selmg, selg, g8[:, 2, :].unsqueeze(2).to_broadcast([128, GT * 8, 8]), ALU.add, [r_sel, r_g8], [r_selm])
            for ti in range(GT):
                k.op("dve", (lambda o=t8[:, ti * 8:(ti + 1) * 8], i=selm[:, ti * NE:(ti + 1) * NE]: (lambda e: e.max(out=o, in_=i)))(),
                     [r_selm], [r_t8])
                k.op("dve", (lambda o=i8u[:, ti * 8:(ti + 1) * 8], m=t8[:, ti * 8:(ti + 1) * 8], v=selm[:, ti * NE:(ti + 1) * NE]:
                             (lambda e: e.max_index(out=o, in_max=m, in_values=v)))(),
                     [r_selm, r_t8], [r_i8u])
            e8g = eidx8f[:, g * GT:(g + 1) * GT, :]
            k.CP("dve", e8g, i8u[:].rearrange("p (t b) -> p t b", b=8), [r_i8u], [r_eidx8f])
            selm3t = selm[:].rearrange("p (t e) -> p t e", e=NE)
            eq3t = eq[:].rearrange("p (t e) -> p t e", e=NE)
            k.TT("dve", eq3t, selm3t, t8v[:, :, 7:8].to_broadcast([128, GT, NE]), ALU.is_ge, [r_selm, r_t8], [r_eq])
            k.CP("pool", Mb[:], eq[:], [r_eq], [r_Mb])
            k.TT("dve", Wgt[:], eq[:], scr[:], ALU.mult, [r_eq, r_scr], [r_Wgt])
            Wgt3 = Wgt[:].rearrange("p (t e) -> p t e", e=NE)
            k.op("dve", (lambda o=wsum[:, 0:GT], i=Wgt3: (lambda e: e.tensor_reduce(out=o, in_=i, axis=AX.X, op=ALU.add)))(),
                 [r_Wgt], [r_wsum])
            k.RCP(wsum[:, 4:4 + GT], wsum[:, 0:GT], [r_wsum], [r_wsum])
            k.TS("dve", wsum[:, 4:4 + GT], wsum[:, 4:4 + GT], 2.5, None, ALU.mult, None, [r_wsum], [r_wsum])
            k.TT("dve", Wgt3, Wgt3, wsum[:, 4:4 + GT].unsqueeze(2).to_broadcast([128, GT, NE]), ALU.mult, [r_Wgt, r_wsum], [r_Wgt])
            for ti in range(GT):
                oh3 = oh[:].rearrange("p (j e) -> p j e", e=NE)
                k.TT("dve", oh3, iota64[:].unsqueeze(1).to_broadcast([128, 8, NE]),
                     eidx8f[:, g * GT + ti, :].unsqueeze(2).to_broadcast([128, 8, NE]), ALU.is_equal, [r_iota, r_eidx8f], [r_oh])
                k.TT("dve", oh3, oh3, Wgt[:, ti * NE:(ti + 1) * NE].unsqueeze(1).to_broadcast([128, 8, NE]), ALU.mult,
                     [r_oh, r_Wgt], [r_oh])
                k.op("dve", (lambda o=W8[:, g * GT + ti, :], i=oh3: (lambda e: e.tensor_reduce(out=o, in_=i, axis=AX.X, op=ALU.add)))(),
                     [r_oh], [r_W8])

        def stageA2b(g):
            pk, rpk = PB[7], RPB[7]
            for ti in range(GT):
                nacc = ti + 1
                k.MM(pk[:, ti * NE:(ti + 1) * NE], tri_bf[:], Mb[:, ti * NE:(ti + 1) * NE], True, nacc == 1, [r_tri, r_Mb], [rpk])
                for tj in range(ti):
                    k.MM(pk[:, ti * NE:(ti + 1) * NE], ones_bf[:], Mb[:, tj * NE:(tj + 1) * NE], False, tj == ti - 1,
                         [r_onesb, r_Mb], [rpk])
            for ti in range(GT):
                k.MM(pk[:, 4 * NE:5 * NE], ones_bf[:], Mb[:, ti * NE:(ti + 1) * NE], ti == 0, ti == GT - 1, [r_onesb, r_Mb], [rpk])
            k.TT("dve", rkc[:, g * GT:(g + 1) * GT, :], pk[:, 0:GT * NE].rearrange("p (t e) -> p t e", e=NE),
                 cnt_run[:].unsqueeze(1).to_broadcast([128, GT, NE]), ALU.add, [rpk, r_cnt], [r_rkc])
            k.TT("dve", cnt_run[:], pk[:, 4 * NE:5 * NE], cnt_run[:], ALU.add, [rpk, r_cnt], [r_cnt])

        def stageB(g):
            x1, r_x1, xs2, r_xs2, fT32, r_fT32, yTg, r_yTg, fTb, r_fTb = bufs(g)
            hsh, r_hsh = hsh_l[g % 2], r_hsh_l[g % 2]
            for fc in range(2):
                pg, rpg = PB[fc * 2], RPB[fc * 2]
                pu, rpu = PB[fc * 2 + 1], RPB[fc * 2 + 1]
                for kk in range(8):
                    k.MM(pg[:, 0:GN], wsh[:, kk * 256 + fc * 128:kk * 256 + fc * 128 + 128], fTb[:, kk, :], kk == 0, kk == 7,
                         [r_wsh, r_fTb[kk]], [rpg])
                for kk in range(8):
                    k.MM(pu[:, 0:GN], wsh[:, 2048 + kk * 256 + fc * 128:2048 + kk * 256 + fc * 128 + 128], fTb[:, kk, :],
                         kk == 0, kk == 7, [r_wsh, r_fTb[kk]], [rpu])
                k.ACT(sgt[fc][:], pg[:, 0:GN], AF.Silu, [rpg], [r_sgt[fc]])
                k.TT("dve", hsh[:, fc, :], pu[:, 0:GN], sgt[fc][:], ALU.mult, [rpu, r_sgt[fc]], [r_hsh])

        def stageB2(g):
            x1, r_x1, xs2, r_xs2, fT32, r_fT32, yTg, r_yTg, fTb, r_fTb = bufs(g)
            hsh, r_hsh = hsh_l[g % 2], r_hsh_l[g % 2]
            for ti in range(GT):
                tok0 = g * GN + ti * 128
                rx1 = r_x1[ti]
                for nn in range(2):
                    pd, rpd = PB[4 + nn], RPB[4 + nn]
                    for fc in range(2):
                        k.MM(pd[:, :], hsh[:, fc, ti * 128:(ti + 1) * 128],
                             wsh[:, 4096 + fc * 1024 + nn * 512:4096 + fc * 1024 + nn * 512 + 512],
                             fc == 0, fc == 1, [r_hsh, r_wsh], [rpd])
                    k.TT("dve", tmpf[:, nn * 512:(nn + 1) * 512], pd[:, :], G2b[:, nn * 512:(nn + 1) * 512], ALU.mult,
                         [rpd, r_G2b], [r_tmpf_h[nn]])
                    k.TT("dve", x1[:, ti, nn * 512:(nn + 1) * 512], x1[:, ti, nn * 512:(nn + 1) * 512],
                         tmpf[:, nn * 512:(nn + 1) * 512], ALU.add, [rx1, r_tmpf_h[nn]], [rx1])
                k.DMA("sp", x1_s[tok0:tok0 + 128, :], x1[:, ti, :], reads=[rx1])

        NG4 = S // GN
        for g in range(NG4 + 3):
            if g < NG4:
                stageA(g)
            if 1 <= g <= NG4:
                stageA2(g - 1)
            if 2 <= g <= NG4 + 1:
                stageB(g - 2)
            if 1 <= g <= NG4:
                stageA2b(g - 1)
            if g >= 3:
                stageB2(g - 3)
        k.barrier()

    with contextlib.ExitStack() as ph:
        pad = k.sbuf("pad", [128, NE], F32, ph); r_pad = R()
        padi = k.sbuf("padi", [128, NE], I32, ph); r_padi = R()
        pend = k.sbuf("pend", [128, NE], F32, ph); r_pend = R()
        pstart = k.sbuf("pstart", [128, NE], F32, ph); r_pstart = R()
        ones64 = k.sbuf("ones64", [128, NE], F32, ph); r_o64 = R()
        cmp = k.sbuf("cmp", [128, 64, NE], F32, ph); r_cmp = R()
        blkE = k.sbuf("blkE", [128, NBLK], F32, ph); r_blkE = R()
        bst = k.sbuf("bst", [128, NBLK], F32, ph); r_bst = R()
        k.DMA("sp", bst[:], bstart_d, writes=[r_bst])
        pidx = k.sbuf("pidx_sb", [128, 1], F32, ph); r_pidx = R()
        k.DMA("sp", pidx[:], pidx_d, writes=[r_pidx])
        k.MS("pool", ones64[:], 1.0, [r_o64])
        k.TS("dve", pad[:], cnt_run[:], 127.0, None, ALU.add, None, [r_cnt], [r_pad])
        k.CP("dve", padi[:], pad[:], [r_pad], [r_padi])
        k.TS("dve", padi[:], padi[:], 7, 7, ALU.arith_shift_right, ALU.logical_shift_left, [r_padi], [r_padi])
        k.CP("dve", pad[:], padi[:], [r_padi], [r_pad])
        k.op("dve", lambda e: e.tensor_tensor_scan(out=pend[:], data0=ones64[:], data1=pad[:], initial=0.0,
                                                   op0=ALU.mult, op1=ALU.add), [r_o64, r_pad], [r_pend])
        k.TT("dve", pstart[:], pend[:], pad[:], ALU.subtract, [r_pend, r_pad], [r_pstart])
        for c0 in range(0, NBLK, 64):
            k.TT("dve", cmp[:], pend[:].unsqueeze(1).to_broadcast([128, 64, NE]),
                 bst[:, c0:c0 + 64].unsqueeze(2).to_broadcast([128, 64, NE]), ALU.is_le, [r_pend, r_bst], [r_cmp])
            k.op("dve", (lambda o=blkE[:, c0:c0 + 64], i=cmp[:]: (lambda e: e.tensor_reduce(out=o, in_=i, axis=AX.X, op=ALU.add)))(),
                 [r_cmp], [r_blkE])
        k.TS("dve", blkE[:], blkE[:], 63.0, None, ALU.min, None, [r_blkE], [r_blkE])
        same = k.sbuf("same", [128, NBLK], F32, ph); r_same = R()
        k.MS("dve", same[:], 0.0, [r_same])
        k.TT("dve", same[:, 1:NBLK], blkE[:, 1:NBLK], blkE[:, 0:NBLK - 1], ALU.is_equal, [r_blkE], [r_same])
        for q in range(1, NWB6):
            k.MS("dve", same[:, q * RUN6:q * RUN6 + 1], 0.0, [r_same])
        k.TS("dve", blkE[:], blkE[:], 128.0, pidx[:, 0:1], ALU.mult, ALU.add, [r_blkE, r_pidx], [r_blkE])
        k.STT(blkE[:], same[:], float(1 << 20), blkE[:], ALU.mult, ALU.add, [r_same, r_blkE], [r_blkE])
        k.CP("dve", idxW[:], blkE[:], [r_blkE], [r_idxW])
        destf = [k.sbuf("destf%d" % i, [128, NE], F32, ph) for i in range(2)]; r_destf = [R() for _ in range(2)]
        d8 = [k.sbuf("d8%d" % i, [128, 8], F32, ph) for i in range(2)]; r_d8 = [R() for _ in range(2)]
        j64b = k.sbuf("j64b", [128, NE], F32, ph); r_j64b = R()
        ft = [k.sbuf("ft%d" % i, [128, D], BF16, ph) for i in range(3)]; r_ft = [R() for _ in range(3)]
        for i in range(32):
            df, rdf = destf[i % 2], r_destf[i % 2]
            dd, rdd = d8[i % 2], r_d8[i % 2]
            k.TT("dve", df[:], rkc[:, i, :], pstart[:], ALU.add, [r_rkc, r_pstart], [rdf])
            k.MS("dve", dd[:], 0.0, [rdd])
            for j in range(8):
                k.op("dve", (lambda o=j64b[:], i0=iota64[:], sc_=eidx8f[:, i, j:j + 1], i1=df[:], a=dd[:, j:j + 1]:
                             (lambda e: e.scalar_tensor_tensor(out=o, in0=i0, scalar=sc_, in1=i1, op0=ALU.is_equal,
                                                               op1=ALU.mult, accum_out=a)))(),
                     [r_iota, r_eidx8f, rdf], [r_j64b, rdd])
            k.CP("dve", dest8u[:, i, :], dd[:], [rdd], [r_dest8u[i]])
            fb_, rfb_ = ft[i % 3], r_ft[i % 3]
            k.DMA("sp", fb_[:], ftok_s[i * 128:(i + 1) * 128, :], reads=[r_ftoks], writes=[rfb_])
            for j in range(8):
                k.dma("pool", (lambda e, src=fb_[:], idx=dest8u[:, i, j:j + 1]:
                               e.indirect_dma_start(out=Xs[:, :], out_offset=bass.IndirectOffsetOnAxis(ap=idx, axis=0),
                                                    in_=src, in_offset=None)),
                      reads=[rfb_, r_dest8u[i]])
        k.barrier()

    with contextlib.ExitStack() as ph:
        NW = NWB6
        wblk = [k.sbuf("wblk%d" % i, [128, 6144], BF16, ph) for i in range(NW)]; r_wblk = [R() for _ in range(NW)]
        xblk = [k.sbuf("xblk%d" % i, [128, D], BF16, ph) for i in range(3)]; r_xblk = [R() for _ in range(3)]
        xT = [k.sbuf("xTb%d" % i, [128, D], BF16, ph) for i in range(2)]; r_xT = [R() for _ in range(2)]
        sgb = [k.sbuf("sgb%d" % i, [128, 256], F32, ph) for i in range(2)]; r_sgb = [R() for _ in range(2)]
        hidb = [k.sbuf("hidb%d" % i, [128, 256], BF16, ph) for i in range(2)]; r_hidb = [R() for _ in range(2)]
        yblk = [k.sbuf("yblk%d" % i, [128, D], F32, ph) for i in range(2)]; r_yblk = [R() for _ in range(2)]

        def sig(b):
            return (b % NW) * RUN6 + b // NW

        bcreg = [None]

        def gather_w(e, o, idx):
            if bcreg[0] is None:
                bcreg[0] = e.alloc_register("bcr")
                e.reg_mov(bcreg[0], NE * 128 - 1)
            return e.indirect_dma_start(out=o, out_offset=None, in_=W_all[:, :],
                                        in_offset=bass.IndirectOffsetOnAxis(ap=idx, axis=0),
                                        bounds_check=bcreg[0], oob_is_err=False)

        def load_w(b):
            wb = b % NW
            k.dma("pool", (lambda e, o=wblk[wb][:], idx=idxW[:, sig(b):sig(b) + 1]: gather_w(e, o, idx)),
                  reads=[r_Wall, r_idxW], writes=[r_wblk[wb]])

        def load_x(b):
            k.DMA("sp", xblk[b % 3][:], Xs[sig(b) * 128:(sig(b) + 1) * 128, :], reads=[r_Xs], writes=[r_xblk[b % 3]])

        hidT = [k.sbuf("hidT%d" % i, [128, 256], BF16, ph) for i in range(2)]; r_hidT = [R() for _ in range(2)]

        def TRb(b):
            pb, rpb = PB[b % 2], RPB[b % 2]
            pbv = pb[:].bitcast(BF16)
            for kk in range(8):
                k.TR(pbv[:, kk * 128:(kk + 1) * 128], xblk[b % 3][:, kk * 128:(kk + 1) * 128], ident_bf[:],
                     [r_xblk[b % 3], r_idb], [rpb])
            if b % 2 == 0:
                k.CP("act", xT[b % 2][:], pbv[:, :], [rpb], [r_xT[b % 2]])
            else:
                k.CP("dve", xT[b % 2][:], pbv[:, :], [rpb], [r_xT[b % 2]])

        def GUb(b):
            wb = b % NW
            pg, rpg = PB[2 + b % 2], RPB[2 + b % 2]
            for kk in range(8):
                k.MM(pg[:, :], xT[b % 2][:, kk * 128:(kk + 1) * 128], wblk[wb][:, kk * 512:(kk + 1) * 512],
                     kk == 0, kk == 7, [r_wblk[wb], r_xT[b % 2]], [rpg])
            k.ACT(sgb[b % 2][:], pg[:, 0:256], AF.Silu, [rpg], [r_sgb[b % 2]])
            k.TT("dve", hidb[b % 2][:], pg[:, 256:512], sgb[b % 2][:], ALU.mult, [rpg, r_sgb[b % 2]], [r_hidb[b % 2]])

        def HTb(b):
            pg, rpg = PB[2 + b % 2], RPB[2 + b % 2]
            pgv = pg[:].bitcast(BF16)
            for fc in range(2):
                k.TR(pgv[:, fc * 128:(fc + 1) * 128], hidb[b % 2][:, fc * 128:(fc + 1) * 128], ident_bf[:],
                     [r_hidb[b % 2], r_idb], [rpg])
            if b % 2 == 0:
                k.CP("dve", hidT[b % 2][:], pgv[:, 0:256], [rpg], [r_hidT[b % 2]])
            else:
                k.CP("act", hidT[b % 2][:], pgv[:, 0:256], [rpg], [r_hidT[b % 2]])

        def DNb(b):
            wb = b % NW
            for nn in range(2):
                pd, rpd = PB[4 + (b % 2) * 2 + nn], RPB[4 + (b % 2) * 2 + nn]
                for fc in range(2):
                    k.MM(pd[:, :], hidT[b % 2][:, fc * 128:(fc + 1) * 128],
                         wblk[wb][:, 4096 + fc * 1024 + nn * 512:4096 + fc * 1024 + nn * 512 + 512],
                         fc == 0, fc == 1, [r_hidT[b % 2], r_wblk[wb]], [rpd])
                if nn == 0:
                    k.CP("act", yblk[b % 2][:, 0:512], pd[:, :], [rpd], [r_yblk[b % 2]])
                else:
                    k.CP("dve", yblk[b % 2][:, 512:1024], pd[:, :], [rpd], [r_yblk[b % 2]])
            k.DMA("sp", Ys[sig(b) * 128:(sig(b) + 1) * 128, :], yblk[b % 2][:], reads=[r_yblk[b % 2]])

        load_x(0)
        load_x(1)
        load_w(0)
        for i in range(NBLK + 3):
            if i + 2 < NBLK:
                load_x(i + 2)
            if i + 1 < NBLK:
                load_w(i + 1)
            if i < NBLK:
                TRb(i)
            if 1 <= i <= NBLK:
                GUb(i - 1)
            if 2 <= i <= NBLK + 1:
                HTb(i - 2)
            if i >= 3:
                DNb(i - 3)
        k.barrier()

    out_toks = []
    with contextlib.ExitStack() as ph:
        fng = k.sbuf("fng", [128, D], F32, ph); r_fng = R()
        k.DMA("sp", fng[:], fng_d, writes=[r_fng])
        yg = [k.sbuf("yg%d" % i, [128, D], F32, ph) for i in range(8)]; r_yg = [R() for _ in range(8)]
        accb = [k.sbuf("accb%d" % i, [128, D], F32, ph) for i in range(2)]; r_accb = [R() for _ in range(2)]
        acp = [k.sbuf("acp%d" % i, [128, D], F32, ph) for i in range(2)]; r_acp = [R() for _ in range(2)]
        xt = [k.sbuf("xt7%d" % i, [128, D], F32, ph) for i in range(2)]; r_xt = [R() for _ in range(2)]
        ot = [k.sbuf("ot7%d" % i, [128, D], F32, ph) for i in range(2)]; r_ot = [R() for _ in range(2)]
        junk5 = k.sbuf("junk7", [128, D], BF16, ph); r_junk5 = R()
        gi_ = 0
        for i in range(32):
            b2 = i % 2
            tok0 = i * 128
            k.DMA("sp", xt[b2][:], x1_s[tok0:tok0 + 128, :], reads=[r_x1s], writes=[r_xt[b2]])
            ac, rac = accb[b2], r_accb[b2]
            for j in range(8):
                gb, rgb = yg[gi_ % 8], r_yg[gi_ % 8]
                gi_ += 1
                k.dma("pool", (lambda e, o=gb[:], idx=dest8u[:, i, j:j + 1]:
                               e.indirect_dma_start(out=o, out_offset=None, in_=Ys[:, :],
                                                    in_offset=bass.IndirectOffsetOnAxis(ap=idx, axis=0))),
                      reads=[r_Ys, r_dest8u[i]], writes=[rgb])
                if j == 0:
                    k.TS("dve", ac[:], gb[:], W8[:, i, 0:1], None, ALU.mult, None, [rgb, r_W8], [rac])
                else:
                    k.STT(ac[:], gb[:], W8[:, i, j:j + 1], ac[:], ALU.mult, ALU.add, [rgb, r_W8, rac], [rac])
            k.TT("dve", ac[:], ac[:], G2b[:], ALU.mult, [rac, r_G2b], [rac])
            k.TT("dve", ac[:], ac[:], xt[b2][:], ALU.add, [rac, r_xt[b2]], [rac])
            ss, rss = new_stat()
            k.ACT(junk5[:], ac[:], AF.Square, [rac], [r_junk5, rss], accum=ss)
            rstd, rrs = rstd_from(ss, rss, D)
            k.STT(ot[b2][:], ac[:], rstd, fng[:], ALU.mult, ALU.mult, [rac, rrs, r_fng], [r_ot[b2]])
            out_toks.append(k.DMA("sp", y_d[tok0:tok0 + 128, :], ot[b2][:], reads=[r_ot[b2]], final=True))

    if "moe" in debug:
        dbg["cnt_run"] = (cnt_run, [128, NE], F32, [r_cnt])
        dbg["eidx8f"] = (eidx8f, [128, 32, 8], F32, [r_eidx8f])
        dbg["W8"] = (W8, [128, 32, 8], F32, [r_W8])
        dbg["dest8u"] = (dest8u, [128, 32, 8], U32, r_dest8u)
        dbg["idxW"] = (idxW, [128, NBLK], U32, [r_idxW])
        dbg["rkc"] = (rkc, [128, 32, NE], F32, [r_rkc])
    dbg_outs = {}
    for name, (tile, shape, dt, rr) in dbg.items():
        dd = nc.dram_tensor("dbg_" + name, list(shape), dt, kind="ExternalOutput").ap()
        out_toks.append(k.DMA("sp", dd, tile[:], reads=rr))
        dbg_outs[name] = "dbg_" + name
    k.barrier()
    k.wait_all("sp", out_toks)
    k.emit()
    k.es.close()
    return nc, dbg_outs


def rope_tables():
    rows = S // 64
    row = np.repeat(np.arange(rows, dtype=np.float32), 64)
    col = np.tile(np.arange(64, dtype=np.float32), rows)
    inv_freq = (10000.0 ** (-np.arange(0, 16, 2, dtype=np.float32) / 16)).astype(np.float32)
    ang = np.stack([row, col], axis=-1)[:, :, None] * inv_freq
    cos = np.cos(ang).astype(np.float32)
    sin = np.sin(ang).astype(np.float32)
    cosT = np.zeros((32, S), np.float32)
    sinT = np.zeros((32, S), np.float32)
    for a in range(2):
        for hf in range(2):
            cosT[a * 16 + hf * 8:a * 16 + hf * 8 + 8, :] = cos[:, a, :].T
            sinT[a * 16 + hf * 8:a * 16 + hf * 8 + 8, :] = sin[:, a, :].T
    return cosT, sinT


def fm(v, nchunks):
    return np.ascontiguousarray(np.asarray(v, np.float32).reshape(nchunks, 128).T)


def make_in_maps(inp):
    g = lambda n: np.asarray(inp[n], dtype=np.float32)
    cosT, sinT = rope_tables()
    shared = {
        "w_mod": np.ascontiguousarray(g("w_mod")[0]),
        "bmT": fm(g("b_mod")[0], 48),
        "bmrow": np.ascontiguousarray(g("b_mod")[0].reshape(1, -1)),
        "nmgT": fm(g("norm_mix_g")[0], 8),
        "nfgT": fm(g("norm_ffn_g")[0], 8),
        "w_in": np.ascontiguousarray(g("w_in")[0]),
        "qngT": fm(g("q_norm_g")[0], 2),
        "w_q_up": np.ascontiguousarray(g("w_q_up")[0]),
        "kvngT": fm(g("kv_norm_g")[0], 1),
        "w_kv_up": np.ascontiguousarray(g("w_kv_up")[0]),
        "cwT": np.ascontiguousarray(g("conv_w")[0].reshape(4, 4, 128).transpose(2, 1, 0)),
        "cbT": fm(g("conv_b")[0], 4),
        "lru_w_a": np.ascontiguousarray(g("lru_w_a")[0]),
        "lru_w_x": np.ascontiguousarray(g("lru_w_x")[0]),
        "lbaT": np.ascontiguousarray(g("lru_b_a")[0].reshape(2, 4, 128).transpose(2, 0, 1)),
        "lbxT": np.ascontiguousarray(g("lru_b_x")[0].reshape(2, 4, 128).transpose(2, 0, 1)),
        "lamT": np.ascontiguousarray(g("lru_lambda")[0].reshape(2, 4, 128).transpose(2, 0, 1)),
        "w_out": np.ascontiguousarray(g("w_out")[0]),
        "router_w": np.ascontiguousarray(g("router_w")[0]),
        "rbias_b": np.ascontiguousarray(np.broadcast_to(g("router_bias")[0][None, :], (128, NE))),
        "exp_w_gate": np.ascontiguousarray(g("exp_w_gate")[0]),
        "exp_w_up": np.ascontiguousarray(g("exp_w_up")[0]),
        "exp_w_down": np.ascontiguousarray(g("exp_w_down")[0]),
        "sh_w_gate": np.ascontiguousarray(g("sh_w_gate")[0]),
        "sh_w_up": np.ascontiguousarray(g("sh_w_up")[0]),
        "sh_w_down": np.ascontiguousarray(g("sh_w_down")[0]),
        "fng_b": np.ascontiguousarray(np.broadcast_to(g("final_norm_g")[None, :], (128, D))),
        "cosT": cosT,
        "sinT": sinT,
        "tri_bf": np.triu(np.ones((128, 128), np.float32), 1).astype(ml_dtypes.bfloat16),
        "iota64": np.ascontiguousarray(np.broadcast_to(np.arange(NE, dtype=np.float32)[None, :], (128, NE))),
        "bstart": np.ascontiguousarray(np.broadcast_to((128.0 * np.arange(NBLK, dtype=np.float32))[None, :], (128, NBLK))),
        "pidx": np.arange(128, dtype=np.float32).reshape(128, 1),
        "nfg_b": np.ascontiguousarray(np.broadcast_to(g("norm_ffn_g")[0][None, :], (128, D))),
        "ident_bf": np.eye(128, dtype=np.float32).astype(ml_dtypes.bfloat16),
        "ident_f": np.eye(128, dtype=np.float32),
    }
    x = g("x")
    c = g("c")
    ctx = g("ctx")
    c_ctx = g("c_ctx")
    maps = []
    for b in range(8):
        m = dict(shared)
        m["x"] = np.ascontiguousarray(x[b])
        m["ctx"] = np.ascontiguousarray(ctx[b])
        cT = np.stack([c[b].reshape(8, 128).T, c_ctx.reshape(8, 128).T], axis=-1)
        m["cT"] = np.ascontiguousarray(cT)
        maps.append(m)
    return maps


_NC_CACHE = {}


def kernel(**inputs):
    if "nc" not in _NC_CACHE:
        _NC_CACHE["nc"] = build_nc()[0]
    nc = _NC_CACHE["nc"]
    maps = make_in_maps(inputs)
    res = run_bass_kernel_spmd(nc, maps, core_ids=list(range(8)))
    out = np.stack([np.asarray(res.results[b]["y"], dtype=np.float32) for b in range(8)], axis=0)
    return out
```

```python
import contextlib
import math
import numpy as np
import ml_dtypes
import concourse.bass as bass
import concourse.mybir as mybir
from concourse.bass_utils import run_bass_kernel_spmd

F32 = mybir.dt.float32
BF16 = mybir.dt.bfloat16
I32 = mybir.dt.int32
U32 = mybir.dt.uint32
ALU = mybir.AluOpType
AF = mybir.ActivationFunctionType
AX = mybir.AxisListType

ENGS = ("pe", "act", "dve", "pool", "sp")

D = 1024
S = 4096
NCTX = 256
T = S + NCTX
NE = 64
EPS = 1e-6
ATTN_SCALE = 96 ** -0.5
NBLK = 320
NSLOT = NBLK * 128
NWB6 = 5
RUN6 = NBLK // NWB6
DEBUG = {}


class R:
    __slots__ = ("name", "w", "rs")

    def __init__(self, name=""):
        self.name = name
        self.w = None
        self.rs = []


class K:
    def __init__(self, nc, n_dma_sems=72):
        self.nc = nc
        self.es = contextlib.ExitStack()
        self.ops = {e: [] for e in ENGS}
        self.nops = {e: 0 for e in ENGS}
        self.signal = {e: set() for e in ENGS}
        self.seen = {e: {} for e in ENGS}
        self.esem = {e: self.es.enter_context(nc.semaphore("s_" + e)) for e in ENGS}
        self.dsems = [self.es.enter_context(nc.semaphore("d%d" % i)) for i in range(n_dma_sems)]
        self.dcnt = [0] * n_dma_sems
        self.dlast = [None] * n_dma_sems
        self.dnext = 0
        self.final_tokens = []
        self.all_dma = []
        self.n_hw = n_dma_sems - 24
        self.swnext = self.n_hw
        self.sw_recs = []

    def sbuf(self, name, shape, dtype, stack=None):
        return (stack or self.es).enter_context(self.nc.sbuf_tensor(name, list(shape), dtype))

    def _deps(self, eng, reads, writes):
        toks = []
        for r in reads:
            if r.w is not None:
                toks.append(r.w)
        for r in writes:
            if r.w is not None:
                toks.append(r.w)
            toks.extend(r.rs)
        return self._mk_waits(eng, toks)

    def _resolve_sw(self, swid):
        rec = self.sw_recs[swid]
        if rec["tok"] is None:
            self.ops["pool"].append(("swdone", [], None, rec["si"]))
            idx = self.nops["pool"]
            self.nops["pool"] += 1
            dummy = self.dummy
            self.ops["pool"].append(("op", [], (lambda e: e.memset(dummy[0:1, 0:1], 0.0)), idx))
            rec["tok"] = ("e", "pool", idx)
        return rec["tok"]

    def _mk_waits(self, eng, toks):
        waits = []
        for t in toks:
            if t[0] == "sw":
                t = self._resolve_sw(t[1])
            if t[0] == "e":
                _, x, n = t
                if x == "pe" and eng == "pe":
                    continue
                if self.seen[eng].get(x, -1) >= n:
                    continue
                self.seen[eng][x] = n
                self.signal[x].add(n)
                waits.append(t)
            else:
                _, si, val = t
                key = ("d", si)
                if self.seen[eng].get(key, -1) >= val:
                    continue
                self.seen[eng][key] = val
                waits.append(t)
        return waits

    def _record(self, tok, reads, writes):
        for r in writes:
            r.w = tok
            r.rs = []
        for r in reads:
            if r not in writes:
                r.rs.append(tok)
                if len(r.rs) > 32:
                    best = {}
                    for t in r.rs:
                        k_ = (t[0], t[1])
                        if k_ not in best or best[k_][2] < t[2]:
                            best[k_] = t
                    r.rs = list(best.values())

    def op(self, eng, fn, reads=(), writes=()):
        waits = self._deps(eng, reads, writes)
        idx = self.nops[eng]
        self.nops[eng] += 1
        self.ops[eng].append(("op", waits, fn, idx))
        tok = ("e", eng, idx)
        self._record(tok, reads, writes)
        return tok

    def dma(self, eng, fn, reads=(), writes=(), final=False):
        if eng == "pool":
            lo, hi = self.n_hw, len(self.dsems)
            si = self.swnext
            self.swnext = lo + (si - lo + 1) % (hi - lo)
        else:
            si = self.dnext
            self.dnext = (self.dnext + 1) % self.n_hw
        waits = self._deps(eng, reads, writes)
        if self.dlast[si] is not None:
            key = ("d", si)
            if self.seen[eng].get(key, -1) < self.dlast[si]:
                self.seen[eng][key] = self.dlast[si]
                waits.append(("d", si, self.dlast[si]))
        self.dcnt[si] += 16
        val = self.dcnt[si]
        self.dlast[si] = val
        self.ops[eng].append(("dma", waits, fn, si))
        tok = ("d", si, val)
        self._record(tok, reads, writes)
        if final:
            self.final_tokens.append(tok)
        return tok

    def barrier(self):
        toks = []
        for swid in range(len(self.sw_recs)):
            self._resolve_sw(swid)
        for x in ENGS:
            if self.nops[x] > 0:
                toks.append(("e", x, self.nops[x] - 1))
        for si in range(len(self.dsems)):
            if self.dlast[si] is not None:
                toks.append(("d", si, self.dlast[si]))
        for e in ENGS:
            waits = self._mk_waits(e, toks)
            if waits:
                self.ops[e].append(("wait", waits, None, None))

    def wait_all(self, eng, toks):
        waits = self._mk_waits(eng, toks)
        if waits:
            self.ops[eng].append(("wait", waits, None, None))

    def emit(self):
        nc = self.nc
        pref = {}
        for e in ENGS:
            sig = self.signal[e]
            c = 0
            arr = []
            for i in range(self.nops[e]):
                if i in sig:
                    c += 1
                arr.append(c)
            pref[e] = arr

        def run(e, engobj):
            for kind, waits, fn, extra in self.ops[e]:
                for t in waits:
                    if t[0] == "e":
                        engobj.wait_ge(self.esem[t[1]], pref[t[1]][t[2]])
                    else:
                        engobj.wait_ge(self.dsems[t[1]], t[2])
                if kind == "op":
                    ins = fn(engobj)
                    if extra in self.signal[e]:
                        ins.then_inc(self.esem[e], 1)
                elif kind == "dma":
                    ins = fn(engobj)
                    ins.then_inc(self.dsems[extra], 16)
                elif kind == "swdma":
                    ins = fn(engobj)
                    ins.then_inc(self.swsems[extra], 16)
                elif kind == "swdone":
                    engobj.wait_ge(self.swsems[extra], 16)
                    if getattr(self, "sw_clear", True):
                        engobj.sem_clear(self.swsems[extra])

        with nc.Block() as block:
            @block.tensor
            def _(e):
                run("pe", e)

            @block.scalar
            def _(e):
                run("act", e)

            @block.vector
            def _(e):
                run("dve", e)

            @block.gpsimd
            def _(e):
                run("pool", e)

            @block.sync
            def _(e):
                run("sp", e)

    def MM(self, out, lhsT, rhs, start, stop, reads, writes):
        return self.op("pe", lambda e: e.matmul(out, lhsT, rhs, start=start, stop=stop), reads, writes)

    def TR(self, out, in_, ident, reads, writes):
        return self.op("pe", lambda e: e.transpose(out, in_, ident), reads, writes)

    def ACT(self, out, in_, func, reads, writes, bias=None, scale=None, accum=None):
        kw = {}
        if bias is not None:
            kw["bias"] = bias
        if scale is not None:
            kw["scale"] = scale
        if accum is not None:
            kw["accum_out"] = accum
        return self.op("act", lambda e: e.activation(out=out, in_=in_, func=func, **kw), reads, writes)

    def TS(self, eng, out, in0, s1, s2, op0, op1, reads, writes):
        if op1 is None:
            return self.op(eng, lambda e: e.tensor_scalar(out=out, in0=in0, scalar1=s1, scalar2=None, op0=op0), reads, writes)
        return self.op(eng, lambda e: e.tensor_scalar(out=out, in0=in0, scalar1=s1, scalar2=s2, op0=op0, op1=op1), reads, writes)

    def TT(self, eng, out, in0, in1, op, reads, writes):
        return self.op(eng, lambda e: e.tensor_tensor(out=out, in0=in0, in1=in1, op=op), reads, writes)

    def STT(self, out, in0, scalar, in1, op0, op1, reads, writes):
        return self.op("dve", lambda e: e.scalar_tensor_tensor(out=out, in0=in0, scalar=scalar, in1=in1, op0=op0, op1=op1), reads, writes)

    def CP(self, eng, out, in_, reads, writes):
        if eng == "act":
            return self.ACT(out, in_, AF.Copy, reads, writes)
        return self.op(eng, lambda e: e.tensor_copy(out, in_), reads, writes)

    def MS(self, eng, ap, val, writes):
        return self.op(eng, lambda e: e.memset(ap, val), (), writes)

    def RCP(self, out, in_, reads, writes):
        return self.op("dve", lambda e: e.reciprocal(out=out, in_=in_), reads, writes)

    def DMA(self, eng, out, in_, reads=(), writes=(), final=False):
        return self.dma(eng, lambda e: e.dma_start(out=out, in_=in_), reads, writes, final=final)


def build_nc(debug=None):
    debug = debug or set()
    nc = bass.Bass("TRN2", target_bir_lowering=False)

    def din(name, shape, dt=F32):
        return nc.dram_tensor(name, list(shape), dt, kind="ExternalInput").ap()

    x_d = din("x", [S, D])
    ctx_d = din("ctx", [NCTX, D])
    cT_d = din("cT", [128, 8, 2])
    wmod_d = din("w_mod", [D, 6 * D])
    bmT_d = din("bmT", [128, 48])
    bmrow_d = din("bmrow", [1, 6 * D])
    nmg_d = din("nmgT", [128, 8])
    nfg_d = din("nfgT", [128, 8])
    win_d = din("w_in", [D, 1440])
    qng_d = din("qngT", [128, 2])
    wq_d = din("w_q_up", [256, 768])
    kvng_d = din("kvngT", [128, 1])
    wkv_d = din("w_kv_up", [128, 1024])
    cw_d = din("cwT", [128, 4, 4])
    cb_d = din("cbT", [128, 4])
    lwa_d = din("lru_w_a", [2, 8, 64, 64])
    lwx_d = din("lru_w_x", [2, 8, 64, 64])
    lba_d = din("lbaT", [128, 2, 4])
    lbx_d = din("lbxT", [128, 2, 4])
    lam_d = din("lamT", [128, 2, 4])
    wout_d = din("w_out", [D, D])
    rw_d = din("router_w", [D, NE])
    rb_d = din("rbias_b", [128, NE])
    eg_d = din("exp_w_gate", [NE, D, 256])
    eu_d = din("exp_w_up", [NE, D, 256])
    ed_d = din("exp_w_down", [NE, 256, D])
    sg_d = din("sh_w_gate", [D, 256])
    su_d = din("sh_w_up", [D, 256])
    sd_d = din("sh_w_down", [256, D])
    fng_d = din("fng_b", [128, D])
    cos_d = din("cosT", [32, S])
    sin_d = din("sinT", [32, S])
    tri_d = din("tri_bf", [128, 128], BF16)
    iota_d = din("iota64", [128, NE])
    bstart_d = din("bstart", [128, NBLK])
    pidx_d = din("pidx", [128, 1])
    nfgb_d = din("nfg_b", [128, D])
    idb_d = din("ident_bf", [128, 128], BF16)
    idf_d = din("ident_f", [128, 128])
    y_d = nc.dram_tensor("y", [S, D], F32, kind="ExternalOutput").ap()

    lx_s = nc.dram_tensor("lx_s", [512, T], F32, kind="Internal").ap()
    lg_s = nc.dram_tensor("lg_s", [512, S], F32, kind="Internal").ap()
    x1_s = nc.dram_tensor("x1_s", [S, D], F32, kind="Internal").ap()
    yT_s = nc.dram_tensor("yT_s", [D, S], BF16, kind="Internal").ap()
    W_all = nc.dram_tensor("W_all", [NE * 128, 6144], BF16, kind="Internal").ap()
    ftok_s = nc.dram_tensor("ftok_s", [S, D], BF16, kind="Internal").ap()
    Xs = nc.dram_tensor("Xs", [NSLOT, D], BF16, kind="Internal").ap()
    Ys = nc.dram_tensor("Ys", [NSLOT, D], F32, kind="Internal").ap()
    fT_s = nc.dram_tensor("fT_s", [D, S], BF16, kind="Internal").ap()

    dbg = {}

    k = K(nc)
    PB = [k.es.enter_context(nc.psum_tensor("pb%d" % i, [128, 512], F32)) for i in range(8)]
    RPB = [R("pb%d" % i) for i in range(8)]

    ones_f = k.sbuf("ones_f", [128, 128], F32); r_ones = R()
    k.MS("pool", ones_f[:], 1.0, [r_ones])
    eps_t = k.sbuf("eps_t", [128, 1], F32); r_eps = R()
    k.MS("pool", eps_t[:], EPS, [r_eps])
    one_t = k.sbuf("one_t", [128, 1], F32); r_one = R()
    k.MS("pool", one_t[:], 1.0, [r_one])
    ident_bf = k.sbuf("ident_bf_sb", [128, 128], BF16); r_idb = R()
    k.DMA("sp", ident_bf[:], idb_d, writes=[r_idb])
    ident_f = k.sbuf("ident_f_sb", [128, 128], F32); r_idf = R()
    k.DMA("sp", ident_f[:], idf_d, writes=[r_idf])

    modT = k.sbuf("modT", [128, 48, 2], F32); r_modT = R()
    A1 = k.sbuf("A1", [128, 8, 2], F32); r_A1 = R()
    A2 = k.sbuf("A2", [128, 8], F32); r_A2 = R()
    G1b = k.sbuf("G1b", [128, D], F32); r_G1b = R()
    G2b = k.sbuf("G2b", [128, D], F32); r_G2b = R()
    stat = k.sbuf("stat", [128, 64], F32)
    stat_r = [R() for _ in range(16)]

    A2b = k.sbuf("A2b", [128, D], F32); r_A2b = R()
    B2b = k.sbuf("B2b", [128, D], F32); r_B2b = R()
    rkc = k.sbuf("rkc", [128, 32, NE], F32); r_rkc = R()
    eidx8f = k.sbuf("eidx8f", [128, 32, 8], F32); r_eidx8f = R()
    W8 = k.sbuf("W8", [128, 32, 8], F32); r_W8 = R()
    dest8u = k.sbuf("dest8u", [128, 32, 8], U32); r_dest8u = [R() for _ in range(32)]
    cnt_run = k.sbuf("cnt_run", [128, NE], F32); r_cnt = R()
    idxW = k.sbuf("idxW", [128, NBLK], U32); r_idxW = R()
    tri_bf = k.sbuf("tri_sb", [128, 128], BF16); r_tri = R()
    k.DMA("sp", tri_bf[:], tri_d, writes=[r_tri])
    ones_bf = k.sbuf("ones_bf", [128, 128], BF16); r_onesb = R()
    k.MS("pool", ones_bf[:], 1.0, [r_onesb])
    iota64 = k.sbuf("iota_sb", [128, NE], F32); r_iota = R()
    k.DMA("sp", iota64[:], iota_d, writes=[r_iota])
    zt = k.sbuf("zt", [128, D], BF16); r_zt = R()
    k.MS("pool", zt[:], 0.0, [r_zt])
    r_Wall = R()
    r_ftoks = R()
    r_Xs = R()
    r_Ys = R()
    r_yTs = R()
    r_fTs = R()
    r_x1s = R()
    r_lxs = R()
    r_lgs = R()

    with contextlib.ExitStack() as ph:
        cT = k.sbuf("cT_sb", [128, 8, 2], F32, ph); r_cT = R()
        k.DMA("sp", cT[:], cT_d, writes=[r_cT])
        sc = k.sbuf("sc", [128, 8, 2], F32, ph); r_sc = R()
        k.ACT(sc[:], cT[:], AF.Silu, [r_cT], [r_sc])
        bmT = k.sbuf("bmT_sb", [128, 48], F32, ph); r_bmT = R()
        k.DMA("sp", bmT[:], bmT_d, writes=[r_bmT])
        bmrow = k.sbuf("bmrow_sb", [1, 6 * D], F32, ph); r_bmrow = R()
        k.DMA("sp", bmrow[:], bmrow_d, writes=[r_bmrow])
        nmg = k.sbuf("nmg", [128, 8], F32, ph); r_nmg = R()
        k.DMA("sp", nmg[:], nmg_d, writes=[r_nmg])
        nfg = k.sbuf("nfg", [128, 8], F32, ph); r_nfg = R()
        k.DMA("sp", nfg[:], nfg_d, writes=[r_nfg])
        grow = k.sbuf("grow", [1, 2, D], F32, ph); r_grow = R()
        grow2 = k.sbuf("grow2", [1, 2, D], F32, ph)
        nfgb = k.sbuf("nfgb", [128, D], F32, ph); r_nfgb = R()
        k.DMA("sp", nfgb[:], nfgb_d, writes=[r_nfgb])
        wm = [k.sbuf("wm%d" % i, [128, 8, 512], F32, ph) for i in range(3)]
        r_wm = [R() for _ in range(3)]
        wmod_v = wmod_d.rearrange("(k p) n -> p k n", p=128)
        psM = PB[0]
        for j in range(12):
            b = j % 3
            k.DMA("sp", wm[b][:], wmod_v[:, :, j * 512:(j + 1) * 512], writes=[r_wm[b]])
            v = j // 2
            if v in (2, 3, 4, 5):
                half = j % 2
                pr = PB[1 + (j % 2)]
                rr = RPB[1 + (j % 2)]
                for kk in range(8):
                    k.MM(pr[0:1, :], sc[:, kk, 0:1], wm[b][:, kk, :], kk == 0, kk == 7, [r_sc, r_wm[b]], [rr])
                tgt = {2: grow[0:1, 0, :], 5: grow[0:1, 1, :], 3: grow2[0:1, 0, :], 4: grow2[0:1, 1, :]}[v]
                k.TT("dve", tgt[:, half * 512:(half + 1) * 512], pr[0:1, :],
                     bmrow[0:1, j * 512:(j + 1) * 512], ALU.add, [rr, r_bmrow], [r_grow])
            if v not in (2, 5):
                for cc in range(4):
                    m = j * 4 + cc
                    for kk in range(8):
                        k.MM(psM[:, m * 2:m * 2 + 2], wm[b][:, kk, cc * 128:(cc + 1) * 128], sc[:, kk, :],
                             kk == 0, kk == 7, [r_sc, r_wm[b]], [RPB[0]])
        psM_v = psM[:, 0:96].rearrange("p (m j) -> p m j", j=2)
        k.MS("pool", modT[:], 0.0, [r_modT])
        for jj in range(2):
            for (m0, m1) in ((0, 16), (24, 40)):
                k.TT("dve", modT[:, m0:m1, jj], psM_v[:, m0:m1, jj], bmT[:, m0:m1], ALU.add, [RPB[0], r_bmT], [r_modT])
        for (srow, Gb, rG) in ((grow[0:1, 0, :], G1b, r_G1b), (grow[0:1, 1, :], G2b, r_G2b),
                               (grow2[0:1, 0, :], B2b, r_B2b), (grow2[0:1, 1, :], A2b, r_A2b)):
            for half in range(2):
                pr = PB[3 + half]
                rr = RPB[3 + half]
                k.MM(pr[:, :], ones_f[0:1, :], srow[:, half * 512:(half + 1) * 512], True, True,
                     [r_ones, r_grow], [rr])
                k.CP("act", Gb[:, half * 512:(half + 1) * 512], pr[:, :], [rr], [rG])
        k.TS("dve", A2b[:], A2b[:], 1.0, None, ALU.add, None, [r_A2b], [r_A2b])
        k.TT("dve", A2b[:], A2b[:], nfgb[:], ALU.mult, [r_A2b, r_nfgb], [r_A2b])
        tmpA = k.sbuf("tmpA", [128, 8, 2], F32, ph); r_tmpA = R()
        k.TS("dve", tmpA[:], modT[:, 8:16, :], 1.0, None, ALU.add, None, [r_modT], [r_tmpA])
        for jj in range(2):
            k.TT("dve", A1[:, :, jj], tmpA[:, :, jj], nmg[:], ALU.mult, [r_tmpA, r_nmg], [r_A1])
        tmpB = k.sbuf("tmpB", [128, 8], F32, ph); r_tmpB = R()
        k.TS("dve", tmpB[:], modT[:, 32:40, 0], 1.0, None, ALU.add, None, [r_modT], [r_tmpB])
        k.TT("dve", A2[:], tmpB[:], nfg[:], ALU.mult, [r_tmpB, r_nfg], [r_A2])
        k.barrier()

    mix = contextlib.ExitStack()
    qlatn = k.sbuf("qlatn", [128, 2, S], BF16, mix); r_qlatn = R()
    kvlatn = k.sbuf("kvlatn", [128, T], BF16, mix); r_kvlatn = R()
    krT = k.sbuf("krT", [96, T], BF16, mix); r_krT = R()

    stat_i = [0]

    def rstd_from(ss_ap, r_ss, n):
        i = stat_i[0] % 16
        stat_i[0] += 1
        col = stat[:, i * 4:i * 4 + 4]
        rr = stat_r[i]
        k.ACT(col[:, 1:2], ss_ap, AF.Sqrt, [r_ss, r_eps], [rr], bias=eps_t[:], scale=1.0 / n)
        k.RCP(col[:, 2:3], col[:, 1:2], [rr], [rr])
        return col[:, 2:3], rr

    def new_stat():
        i = stat_i[0] % 16
        return stat[:, i * 4:i * 4 + 1], stat_r[i]

    groups = [(0, NCTX, True)] + [(NCTX + g * 512, 512, False) for g in range(8)]
    with contextlib.ExitStack() as ph:
        win = k.sbuf("win", [128, 8, 1440], BF16, ph); r_win = R()
        win_v = win_d.rearrange("(k p) n -> p k n", p=128)
        for kk in range(8):
            k.DMA("pool", win[:, kk, :], win_v[:, kk, :], writes=[r_win])
        wkr = k.sbuf("wkr", [128, 8, 96], BF16, ph); r_wkr = R()
        wkrs = k.sbuf("wkrs", [128, 8, 96], BF16, ph); r_wkrs = R()
        k.MS("pool", wkr[:], 0.0, [r_wkr])
        k.MS("pool", wkrs[:], 0.0, [r_wkrs])
        k.CP("pool", wkr[:, :, 64:96], win[:, :, 384:416], [r_win], [r_wkr])
        for a in range(2):
            k.TS("pool", wkrs[:, :, 64 + a * 16:64 + a * 16 + 8], win[:, :, 384 + a * 16 + 8:384 + a * 16 + 16],
                 -1.0, None, ALU.mult, None, [r_win], [r_wkrs])
            k.CP("pool", wkrs[:, :, 64 + a * 16 + 8:64 + a * 16 + 16], win[:, :, 384 + a * 16:384 + a * 16 + 8],
                 [r_win], [r_wkrs])
        cs = [k.sbuf("cs%d" % i, [96, 2, 512], F32, ph) for i in range(2)]
        r_cs = [R() for _ in range(2)]

        xt = [k.sbuf("xt%d" % i, [128, D], F32, ph) for i in range(3)]
        r_xt = [R() for _ in range(3)]
        xs = [k.sbuf("xs%d" % i, [128, 4, D], BF16, ph) for i in range(2)]
        r_xs = [R() for _ in range(2)]
        junk = k.sbuf("junk", [128, D], BF16, ph); r_junk = R()
        hT = [k.sbuf("hT%d" % i, [128, 8, 512], BF16, ph) for i in range(2)]
        r_hT = [[R() for _ in range(8)] for _ in range(2)]
        stg = [k.sbuf("stg%d" % i, [128, 512], F32, ph) for i in range(4)]
        r_stg = [R() for _ in range(4)]
        qraw = [k.sbuf("qraw%d" % i, [128, 512], F32, ph) for i in range(3)]
        r_qraw = [R() for _ in range(3)]
        sq = [k.sbuf("sq%d" % i, [128, 512], F32, ph) for i in range(3)]
        r_sq = [R() for _ in range(3)]
        rbc = k.sbuf("rbc", [128, 512], F32, ph); r_rbc = R()
        rt1 = k.sbuf("rt1", [96, 512], F32, ph); r_rt1 = R()
        rt2 = k.sbuf("rt2", [96, 512], F32, ph); r_rt2 = R()
        xti = 0
        stgi = 0
        qri = [0]
        for gi, (t0, n, is_ctx) in enumerate(groups):
            nt = n // 128
            j = 1 if is_ctx else 0
            xb = xs[gi % 2]
            rxb = r_xs[gi % 2]
            hb = hT[gi % 2]
            rhb = r_hT[gi % 2]
            for ti in range(nt):
                b = xti % 3
                xti += 1
                src = ctx_d[ti * 128:(ti + 1) * 128, :] if is_ctx else x_d[t0 - NCTX + ti * 128:t0 - NCTX + (ti + 1) * 128, :]
                k.DMA("sp", xt[b][:], src, writes=[r_xt[b]])
                ss, rss = new_stat()
                k.ACT(junk[:], xt[b][:], AF.Square, [r_xt[b]], [r_junk, rss], accum=ss)
                rstd, rrs = rstd_from(ss, rss, D)
                k.ACT(xb[:, ti, :], xt[b][:], AF.Copy, [r_xt[b], rrs], [rxb], scale=rstd)
            for kk in range(8):
                pb = PB[kk % 2]
                rpb = RPB[kk % 2]
                pbv = pb[:].bitcast(BF16)
                for ti in range(nt):
                    k.TR(pbv[:, ti * 128:(ti + 1) * 128], xb[:, ti, kk * 128:(kk + 1) * 128], ident_bf[:],
                         [rxb, r_idb], [rpb])
                eng = "dve" if kk % 2 == 0 else "pool"
                if eng == "pool":
                    k.ACT(hb[:, kk, 0:n], pbv[:, 0:n], AF.Identity, [rpb, r_A1, r_modT], [rhb[kk]],
                          bias=modT[:, kk, j:j + 1], scale=A1[:, kk, j:j + 1])
                else:
                    k.TS("dve", hb[:, kk, 0:n], pbv[:, 0:n], A1[:, kk, j:j + 1], modT[:, kk, j:j + 1],
                         ALU.mult, ALU.add, [rpb, r_A1, r_modT], [rhb[kk]])
            pbi = [2]

            def nextpb():
                i = pbi[0]
                pbi[0] = 2 + (pbi[0] - 2 + 1) % 5
                return PB[i], RPB[i]

            def proj(cols0, m, lhs_t=None, r_l=None):
                pb, rpb = nextpb()
                for kk in range(8):
                    lhsT = win[:, kk, cols0:cols0 + m] if lhs_t is None else lhs_t[:, kk, 0:m]
                    k.MM(pb[0:m, 0:n], lhsT, hb[:, kk, 0:n], kk == 0, kk == 7,
                         [r_win if r_l is None else r_l, rhb[kk]], [rpb])
                return pb, rpb

            def latent_norm(cols_list, nfeat, dst_fn, r_dst):
                raws = []
                for ci, c0 in enumerate(cols_list):
                    pb, rpb = proj(c0, 128)
                    q = qri[0] % 3
                    qri[0] += 1
                    k.CP("act", qraw[q][:, 0:n], pb[:, 0:n], [rpb], [r_qraw[q]])
                    k.ACT(sq[q][:, 0:n], pb[:, 0:n], AF.Square, [rpb], [r_sq[q]])
                    raws.append(q)
                pbs, rpbs = nextpb()
                for ci, q in enumerate(raws):
                    k.MM(pbs[:, 0:n], ones_f[:], sq[q][:, 0:n], ci == 0, ci == len(raws) - 1, [r_ones, r_sq[q]], [rpbs])
                k.ACT(rbc[:, 0:n], pbs[:, 0:n], AF.Sqrt, [rpbs, r_eps], [r_rbc], bias=eps_t[:], scale=1.0 / nfeat)
                k.RCP(rbc[:, 0:n], rbc[:, 0:n], [r_rbc], [r_rbc])
                for ci, q in enumerate(raws):
                    k.TT("dve", dst_fn(ci), qraw[q][:, 0:n], rbc[:, 0:n], ALU.mult, [r_qraw[q], r_rbc], [r_dst])

            if not is_ctx:
                q0 = t0 - NCTX
                latent_norm([0, 128], 256, lambda ci: qlatn[:, ci, q0:q0 + n], r_qlatn)
            latent_norm([256], 128, lambda ci: kvlatn[:, t0:t0 + n], r_kvlatn)
            pb1, rpb1 = proj(0, 96, wkr, r_wkr)
            if is_ctx:
                k.CP("act", krT[64:96, t0:t0 + n], pb1[64:96, 0:n], [rpb1], [r_krT])
            else:
                pb2, rpb2 = proj(0, 96, wkrs, r_wkrs)
                q0 = t0 - NCTX
                csb, rcsb = cs[gi % 2], r_cs[gi % 2]
                k.DMA("sp", csb[64:96, 0, :], cos_d[:, q0:q0 + n], writes=[rcsb])
                k.DMA("sp", csb[64:96, 1, :], sin_d[:, q0:q0 + n], writes=[rcsb])
                k.TT("dve", rt1[64:96, 0:n], pb1[64:96, 0:n], csb[64:96, 0, 0:n], ALU.mult, [rpb1, rcsb], [r_rt1])
                k.TT("dve", rt2[64:96, 0:n], pb2[64:96, 0:n], csb[64:96, 1, 0:n], ALU.mult, [rpb2, rcsb], [r_rt2])
                k.TT("pool", krT[64:96, t0:t0 + n], rt1[64:96, 0:n], rt2[64:96, 0:n], ALU.add, [r_rt1, r_rt2], [r_krT])
            for c in range(4):
                pb, rpb = proj(416 + c * 128, 128)
                sb = stgi % 4
                stgi += 1
                k.CP("act", stg[sb][:, 0:n], pb[:, 0:n], [rpb], [r_stg[sb]])
                k.DMA("pool", lx_s[c * 128:(c + 1) * 128, t0:t0 + n], stg[sb][:, 0:n], reads=[r_stg[sb]])
            if not is_ctx:
                q0 = t0 - NCTX
                for c in range(4):
                    pb, rpb = proj(928 + c * 128, 128)
                    sb = stgi % 4
                    stgi += 1
                    k.ACT(stg[sb][:, 0:n], pb[:, 0:n], AF.Gelu_apprx_tanh, [rpb], [r_stg[sb]])
                    k.DMA("pool", lg_s[c * 128:(c + 1) * 128, q0:q0 + n], stg[sb][:, 0:n], reads=[r_stg[sb]])
        k.barrier()

    if "p1" in debug:
        dbg["qlatn"] = (qlatn, [128, 2, S], BF16, [r_qlatn])
        dbg["kvlatn"] = (kvlatn, [128, T], BF16, [r_kvlatn])
        dbg["krT"] = (krT, [96, T], BF16, [r_krT])

    with contextlib.ExitStack() as ph:
        wbd = k.sbuf("wbd", [128, 2, 2, 4, 128], BF16, ph); r_wbd = R()
        k.MS("pool", wbd[:], 0.0, [r_wbd])
        for d in range(2):
            for gsel, wsrc in enumerate((lwa_d, lwx_d)):
                for c in range(4):
                    for hb_ in range(2):
                        k.DMA("pool", wbd[hb_ * 64:(hb_ + 1) * 64, d, gsel, c, hb_ * 64:(hb_ + 1) * 64],
                              wsrc[d, 2 * c + hb_, :, :], writes=[r_wbd])
        cw = k.sbuf("cw", [128, 4, 4], F32, ph); r_cw = R()
        k.DMA("sp", cw[:], cw_d, writes=[r_cw])
        cb = k.sbuf("cb", [128, 4], F32, ph)
        k.DMA("sp", cb[:], cb_d, writes=[r_cw])
        lba = k.sbuf("lba", [128, 2, 4], F32, ph)
        lbx = k.sbuf("lbx", [128, 2, 4], F32, ph)
        lam = k.sbuf("lam", [128, 2, 4], F32, ph); r_lam = R()
        k.DMA("sp", lba[:], lba_d, writes=[r_cw])
        k.DMA("sp", lbx[:], lbx_d, writes=[r_cw])
        nlba = k.sbuf("nlba", [128, 2, 4], F32, ph)
        nlbx = k.sbuf("nlbx", [128, 2, 4], F32, ph)
        k.TS("dve", nlba[:], lba[:], -1.0, None, ALU.mult, None, [r_cw], [r_cw])
        k.TS("dve", nlbx[:], lbx[:], -1.0, None, ALU.mult, None, [r_cw], [r_cw])
        k.DMA("sp", lam[:], lam_d, writes=[r_lam])
        cneg = k.sbuf("cneg", [128, 2, 4], F32, ph); r_cneg = R()
        k.ACT(cneg[:], lam[:], AF.Exp, [r_lam], [r_cneg], scale=-1.0)
        k.ACT(cneg[:], cneg[:], AF.Ln, [r_cneg, r_one], [r_cneg], bias=one_t[:], scale=1.0)
        k.TS("dve", cneg[:], cneg[:], -8.0, None, ALU.mult, None, [r_cneg], [r_cneg])

        xc = k.sbuf("xc", [128, T], F32, ph); r_xc = R()
        u = k.sbuf("u", [128, T], F32, ph); r_u = R()
        ub = k.sbuf("ub", [128, T], BF16, ph); r_ub = R()
        hsum = k.sbuf("hsum", [128, S], F32, ph); r_hsum = R()
        gc = k.sbuf("gc", [128, S], F32, ph); r_gc = R()
        NB = 4
        rg = [k.sbuf("rg%d" % i, [128, 512], F32, ph) for i in range(NB)]; r_rg = [R() for _ in range(NB)]
        ig = [k.sbuf("ig%d" % i, [128, 512], F32, ph) for i in range(NB)]; r_ig = [R() for _ in range(NB)]
        a2 = [k.sbuf("a2%d" % i, [128, 512], F32, ph) for i in range(NB)]; r_a2 = [R() for _ in range(NB)]
        hg = [k.sbuf("hg%d" % i, [128, 512], F32, ph) for i in range(NB)]; r_hg = [R() for _ in range(NB)]
        tsum = k.sbuf("tsum", [128, 512], F32, ph); r_tsum = R()
        yst = [k.sbuf("yst%d" % i, [128, 512], BF16, ph) for i in range(2)]; r_yst = [R() for _ in range(2)]
        ysi = 0
        segs = [(0, NCTX), (NCTX, T)]
        it = 0
        for c in range(4):
            k.DMA("sp", xc[:], lx_s[c * 128:(c + 1) * 128, :], reads=[r_lxs], writes=[r_xc])
            k.DMA("sp", gc[:], lg_s[c * 128:(c + 1) * 128, :], reads=[r_lgs], writes=[r_gc])
            for zb in range(c * (NBLK // 4), (c + 1) * (NBLK // 4)):
                k.DMA("sp", Xs[zb * 128:(zb + 1) * 128, :], zt[:], reads=[r_zt])
            for (s0, e0) in segs:
                k.TS("dve", u[:, s0:e0], xc[:, s0:e0], cw[:, c, 2:3], cb[:, c:c + 1], ALU.mult, ALU.add,
                     [r_xc, r_cw], [r_u])
                k.STT(u[:, s0 + 2:e0], xc[:, s0:e0 - 2], cw[:, c, 0:1], u[:, s0 + 2:e0], ALU.mult, ALU.add,
                      [r_xc, r_cw, r_u], [r_u])
                k.STT(u[:, s0 + 1:e0], xc[:, s0:e0 - 1], cw[:, c, 1:2], u[:, s0 + 1:e0], ALU.mult, ALU.add,
                      [r_xc, r_cw, r_u], [r_u])
                k.STT(u[:, s0:e0 - 1], xc[:, s0 + 1:e0], cw[:, c, 3:4], u[:, s0:e0 - 1], ALU.mult, ALU.add,
                      [r_xc, r_cw, r_u], [r_u])
            k.CP("pool", ub[:], u[:], [r_u], [r_ub])
            for d in range(2):
                order = [groups[0]] + (groups[1:] if d == 0 else groups[1:][::-1])
                prev_init = [None]

                def st1(grp, b):
                    (t0, n, is_ctx) = grp
                    pa, rpa = PB[(b * 2) % 8], RPB[(b * 2) % 8]
                    px, rpx = PB[(b * 2 + 1) % 8], RPB[(b * 2 + 1) % 8]
                    k.MM(pa[:, 0:n], wbd[:, d, 0, c, :], ub[:, t0:t0 + n], True, True, [r_wbd, r_ub], [rpa])
                    k.MM(px[:, 0:n], wbd[:, d, 1, c, :], ub[:, t0:t0 + n], True, True, [r_wbd, r_ub], [rpx])
                    k.ACT(rg[b][:, 0:n], pa[:, 0:n], AF.Sigmoid, [rpa, r_cw], [r_rg[b]], bias=lba[:, d, c:c + 1])
                    k.ACT(ig[b][:, 0:n], px[:, 0:n], AF.Sigmoid, [rpx, r_cw], [r_ig[b]], bias=lbx[:, d, c:c + 1])

                def st2(grp, b):
                    (t0, n, is_ctx) = grp
                    k.ACT(rg[b][:, 0:n], rg[b][:, 0:n], AF.Exp, [r_rg[b], r_cneg], [r_rg[b]], scale=cneg[:, d, c:c + 1])
                    k.TT("pool", a2[b][:, 0:n], rg[b][:, 0:n], rg[b][:, 0:n], ALU.mult, [r_rg[b]], [r_a2[b]])
                    k.TT("dve", ig[b][:, 0:n], ig[b][:, 0:n], u[:, t0:t0 + n], ALU.mult, [r_ig[b], r_u], [r_ig[b]])

                def st3(grp, b):
                    (t0, n, is_ctx) = grp
                    k.ACT(a2[b][:, 0:n], a2[b][:, 0:n], AF.Sqrt, [r_a2[b], r_one], [r_a2[b]], bias=one_t[:], scale=-1.0)
                    k.TT("dve", a2[b][:, 0:n], a2[b][:, 0:n], ig[b][:, 0:n], ALU.mult, [r_a2[b], r_ig[b]], [r_a2[b]])

                def st4(grp, b):
                    nonlocal ysi
                    (t0, n, is_ctx) = grp
                    if d == 0:
                        o_ap, a_ap, b_ap = hg[b][:, 0:n], rg[b][:, 0:n], a2[b][:, 0:n]
                    else:
                        o_ap, a_ap, b_ap = hg[b][:, 0:n][:, ::-1], rg[b][:, 0:n][:, ::-1], a2[b][:, 0:n][:, ::-1]
                    rds = [r_rg[b], r_a2[b]]
                    if prev_init[0] is None:
                        init = 0.0
                    else:
                        init = prev_init[0][0]
                        rds.append(prev_init[0][1])
                    k.op("dve", (lambda o_ap=o_ap, a_ap=a_ap, b_ap=b_ap, init=init:
                                 (lambda e: e.tensor_tensor_scan(out=o_ap, data0=a_ap, data1=b_ap, initial=init,
                                                                 op0=ALU.mult, op1=ALU.add)))(),
                         rds, [r_hg[b]])
                    last = hg[b][:, n - 1:n] if d == 0 else hg[b][:, 0:1]
                    prev_init[0] = (last, r_hg[b])
                    if not is_ctx:
                        q0 = t0 - NCTX
                        if d == 0:
                            k.CP("pool", hsum[:, q0:q0 + n], hg[b][:, 0:n], [r_hg[b]], [r_hsum])
                        else:
                            k.TT("dve", tsum[:, 0:n], hg[b][:, 0:n], hsum[:, q0:q0 + n], ALU.add, [r_hg[b], r_hsum], [r_tsum])
                            yb, ryb = yst[ysi % 2], r_yst[ysi % 2]
                            ysi += 1
                            k.TT("pool", yb[:, 0:n], tsum[:, 0:n], gc[:, q0:q0 + n], ALU.mult,
                                 [r_tsum, r_gc], [ryb])
                            k.DMA("sp", yT_s[(4 + c) * 128:(5 + c) * 128, q0:q0 + n], yb[:, 0:n], reads=[ryb])

                for p0 in range(0, len(order), 2):
                    pair = [(order[p0 + q], ((p0 // 2) % 2) * 2 + q) for q in range(2) if p0 + q < len(order)]
                    for (grp, b) in pair:
                        st1(grp, b)
                    for (grp, b) in pair:
                        st2(grp, b)
                    for (grp, b) in pair:
                        st3(grp, b)
                    for (grp, b) in pair:
                        st4(grp, b)
        k.barrier()

    with contextlib.ExitStack() as ph:
        wq = k.sbuf("wq", [128, 2, 768], BF16, ph); r_wq = R()
        wqs = k.sbuf("wqs", [128, 2, 768], BF16, ph); r_wqs = R()
        wkv = k.sbuf("wkv", [128, 1024], BF16, ph); r_wkv = R()
        with contextlib.ExitStack() as ph2:
            wtmp = k.sbuf("wtmp", [128, 2, 1024], F32, ph2); r_wtmp = R()
            qng = k.sbuf("qng", [128, 2], F32, ph2); r_qng = R()
            kvng = k.sbuf("kvng", [128, 1], F32, ph2)
            k.DMA("sp", qng[:], qng_d, writes=[r_qng])
            k.DMA("sp", kvng[:], kvng_d, writes=[r_qng])
            wq_v = wq_d.rearrange("(k p) n -> p k n", p=128)
            k.DMA("sp", wtmp[:, :, 0:768], wq_v, writes=[r_wtmp])
            for m in range(2):
                k.TS("dve", wq[:, m, :], wtmp[:, m, 0:768], qng[:, m:m + 1], None, ALU.mult, None, [r_wtmp, r_qng], [r_wq])
            k.MS("pool", wqs[:], 0.0, [r_wqs])
            for h in range(8):
                for a in range(2):
                    base = h * 96 + 64 + a * 16
                    k.TS("pool", wqs[:, :, base:base + 8], wq[:, :, base + 8:base + 16], -1.0, None, ALU.mult, None,
                         [r_wq], [r_wqs])
                    k.CP("pool", wqs[:, :, base + 8:base + 16], wq[:, :, base:base + 8], [r_wq], [r_wqs])
            wtmp2 = k.sbuf("wtmp2", [128, 1024], F32, ph2); r_wtmp2 = R()
            k.DMA("sp", wtmp2[:], wkv_d, writes=[r_wtmp2])
            k.TS("dve", wkv[:], wtmp2[:], kvng[:, 0:1], None, ALU.mult, None, [r_wtmp2, r_qng], [r_wkv])
            k.barrier()
        cs = [k.sbuf("cs3%d" % i, [96, 2, 512], F32, ph) for i in range(2)]
        r_cs = [R() for _ in range(2)]
        csi = [0]
        yst = [k.sbuf("yst3%d" % i, [64, 512], BF16, ph) for i in range(2)]; r_yst = [R() for _ in range(2)]
        wst = [k.sbuf("wst%d" % i, [128, 6144], BF16, ph) for i in range(2)]; r_wst = [R() for _ in range(2)]

        def precast(e):
            b_ = e % 2
            gu_v = wst[b_][:, 0:4096].rearrange("p (k t f) -> p k t f", t=2, f=256)
            k.DMA("pool", gu_v[:, :, 0, :], eg_d[e].rearrange("(k p) f -> p k f", p=128), writes=[r_wst[b_]])
            k.DMA("pool", gu_v[:, :, 1, :], eu_d[e].rearrange("(k p) f -> p k f", p=128), writes=[r_wst[b_]])
            k.DMA("pool", wst[b_][:, 4096:6144].rearrange("p (k f) -> p k f", f=1024),
                  ed_d[e].rearrange("(k p) f -> p k f", p=128), writes=[r_wst[b_]])
            k.DMA("sp", W_all[e * 128:(e + 1) * 128, :], wst[b_][:], reads=[r_wst[b_]])

        Qh = [k.sbuf("Qh%d" % i, [96, S], BF16, ph) for i in range(2)]; r_Qh = [R() for _ in range(2)]
        Kh = [k.sbuf("Kh%d" % i, [96, T], BF16, ph) for i in range(2)]; r_Kh = [R() for _ in range(2)]
        Va = [k.sbuf("Va%d" % i, [128, 34, 128], BF16, ph) for i in range(2)]; r_Va = [R() for _ in range(2)]
        for i in range(2):
            k.MS("pool", Va[i][:, :, 64:128], 1.0, [r_Va[i]])
        rt1 = k.sbuf("rt1b", [96, 512], F32, ph); r_rt1 = R()
        rt2 = k.sbuf("rt2b", [96, 512], F32, ph); r_rt2 = R()
        PT = [k.sbuf("PT%d" % i, [128, 512], BF16, ph) for i in range(3)]; r_PT = [R() for _ in range(3)]
        rcp = [k.sbuf("rcp%d" % i, [128, 512], F32, ph) for i in range(2)]; r_rcp = [R() for _ in range(2)]
        misc = [5, 6, 7]
        mi = [0]

        def mpb():
            i = misc[mi[0] % 3]
            mi[0] += 1
            return PB[i], RPB[i]

        def build_head(h):
            s = h % 2
            for g in range(8):
                c0 = g * 512
                p1, rp1 = mpb()
                p2, rp2 = mpb()
                for m in range(2):
                    k.MM(p1[0:96, :], wq[:, m, h * 96:(h + 1) * 96], qlatn[:, m, c0:c0 + 512], m == 0, m == 1,
                         [r_wq, r_qlatn], [rp1])
                for m in range(2):
                    k.MM(p2[0:96, :], wqs[:, m, h * 96:(h + 1) * 96], qlatn[:, m, c0:c0 + 512], m == 0, m == 1,
                         [r_wqs, r_qlatn], [rp2])
                k.CP("dve", Qh[s][0:64, c0:c0 + 512], p1[0:64, :], [rp1], [r_Qh[s]])
                csb, rcsb = cs[csi[0] % 2], r_cs[csi[0] % 2]
                csi[0] += 1
                k.DMA("sp", csb[64:96, 0, :], cos_d[:, c0:c0 + 512], writes=[rcsb])
                k.DMA("sp", csb[64:96, 1, :], sin_d[:, c0:c0 + 512], writes=[rcsb])
                k.TT("dve", rt1[64:96, :], p1[64:96, :], csb[64:96, 0, :], ALU.mult, [rp1, rcsb], [r_rt1])
                k.TT("dve", rt2[64:96, :], p2[64:96, :], csb[64:96, 1, :], ALU.mult, [rp2, rcsb], [r_rt2])
                k.TT("pool", Qh[s][64:96, c0:c0 + 512], rt1[64:96, :], rt2[64:96, :], ALU.add, [r_rt1, r_rt2], [r_Qh[s]])
            for (t0, n, _) in groups:
                p1, rp1 = mpb()
                k.MM(p1[0:64, 0:n], wkv[:, h * 128:h * 128 + 64], kvlatn[:, t0:t0 + n], True, True,
                     [r_wkv, r_kvlatn], [rp1])
                k.CP("dve", Kh[s][0:64, t0:t0 + n], p1[0:64, 0:n], [rp1], [r_Kh[s]])
            k.CP("pool", Kh[s][64:96, :], krT[64:96, :], [r_krT], [r_Kh[s]])
            for kt0 in range(0, 34, 8):
                nk = min(8, 34 - kt0)
                p1, rp1 = mpb()
                for i in range(nk):
                    kt = kt0 + i
                    k.MM(p1[:, i * 64:(i + 1) * 64], kvlatn[:, kt * 128:(kt + 1) * 128],
                         wkv[:, h * 128 + 64:h * 128 + 128], True, True, [r_wkv, r_kvlatn], [rp1])
                k.CP("dve", Va[s][:, kt0:kt0 + nk, 0:64], p1[:, 0:nk * 64].rearrange("p (a b) -> p a b", b=64),
                     [rp1], [r_Va[s]])

        def attend(h):
            s = h % 2
            seq = [(qg, kt) for qg in range(8) for kt in range(34)]
            LA = 2

            def issue_S(i):
                qg, kt = seq[i]
                c0 = qg * 512
                if kt == 0:
                    precast(h * 8 + qg)
                ps_, rps = PB[i % 3], RPB[i % 3]
                k.MM(ps_[:, :], Kh[s][:, kt * 128:(kt + 1) * 128], Qh[s][:, c0:c0 + 512], True, True,
                     [r_Kh[s], r_Qh[s]], [rps])

            def issue_rest(i):
                qg, kt = seq[i]
                c0 = qg * 512
                ps_, rps = PB[i % 3], RPB[i % 3]
                po, rpo = PB[3 + (qg % 2)], RPB[3 + (qg % 2)]
                pt, rpt = PT[i % 3], r_PT[i % 3]
                k.ACT(pt[:], ps_[:, :], AF.Exp, [rps], [rpt], scale=ATTN_SCALE)
                k.MM(po[:, :], Va[s][:, kt, :], pt[:], kt == 0, kt == 33, [r_Va[s], rpt], [rpo])
                if kt == 33:
                    rc, rrc = rcp[qg % 2], r_rcp[qg % 2]
                    k.RCP(rc[64:128, :], po[64:128, :], [rpo], [rrc])
                    yb, ryb = yst[qg % 2], r_yst[qg % 2]
                    k.TT("dve", yb[0:64, :], po[0:64, :], rc[64:128, :], ALU.mult, [rpo, rrc], [ryb])
                    k.DMA("sp", yT_s[h * 64:(h + 1) * 64, c0:c0 + 512], yb[0:64, :], reads=[ryb])

            for i in range(len(seq) + LA):
                if i < len(seq):
                    issue_S(i)
                if i >= LA:
                    issue_rest(i - LA)

        build_head(0)
        for h in range(8):
            if h + 1 < 8:
                build_head(h + 1)
            attend(h)
        k.barrier()

    mix.close()

    with contextlib.ExitStack() as ph:
        wout = k.sbuf("wout", [128, 8, D], BF16, ph); r_wout = R()
        wout_v = wout_d.rearrange("(k p) n -> p k n", p=128)
        for kk in range(8):
            k.DMA("pool", wout[:, kk, :], wout_v[:, kk, :], writes=[r_wout])
        wsh = k.sbuf("wsh", [128, 6144], BF16, ph); r_wsh = R()
        k.DMA("pool", wsh[:, 0:2048].rearrange("p (k f) -> p k f", f=256), sg_d.rearrange("(k p) f -> p k f", p=128), writes=[r_wsh])
        k.DMA("pool", wsh[:, 2048:4096].rearrange("p (k f) -> p k f", f=256), su_d.rearrange("(k p) f -> p k f", p=128), writes=[r_wsh])
        k.DMA("pool", wsh[:, 4096:6144].rearrange("p (k f) -> p k f", f=1024), sd_d.rearrange("(k p) f -> p k f", p=128), writes=[r_wsh])
        rw = k.sbuf("rw", [128, 8, NE], F32, ph); r_rw = R()
        k.DMA("sp", rw[:], rw_d.rearrange("(k p) n -> p k n", p=128), writes=[r_rw])
        rbias = k.sbuf("rbias", [128, NE], F32, ph); r_rbias = R()
        k.DMA("sp", rbias[:], rb_d, writes=[r_rbias])
        xt = [k.sbuf("xt4%d" % i, [128, D], F32, ph) for i in range(4)]; r_xt = [R() for _ in range(4)]
        GT = 2
        GN = GT * 128
        x1_l = [k.sbuf("x1b%d" % i, [128, GT, D], F32, ph) for i in range(4)]; r_x1_l = [[R() for _ in range(GT)] for _ in range(4)]
        xs2_l = [k.sbuf("xs2b%d" % i, [128, GT, D], F32, ph) for i in range(2)]; r_xs2_l = [[R() for _ in range(GT)] for _ in range(2)]
        fT32_l = [k.sbuf("fT32b%d" % i, [128, 8, GN], F32, ph) for i in range(2)]; r_fT32_l = [[R() for _ in range(8)] for _ in range(2)]
        yTg_l = [k.sbuf("yTg%d" % i, [128, 8, GN], BF16, ph) for i in range(2)]; r_yTg_l = [R() for _ in range(2)]
        fTb_l = [k.sbuf("fTb%d" % i, [128, 8, GN], BF16, ph) for i in range(2)]; r_fTb_l = [[R() for _ in range(8)] for _ in range(2)]
        tmpf = k.sbuf("tmpf", [128, D], F32, ph); r_tmpf = R(); r_tmpf_h = [R(), R()]
        tmpf2 = k.sbuf("tmpf2", [128, D], F32, ph); r_tmpf2 = R()
        ftk = [k.sbuf("ftk%d" % i, [128, D], BF16, ph) for i in range(2)]; r_ftk = [R() for _ in range(2)]
        sgt = [k.sbuf("sgt4%d" % i, [128, GN], F32, ph) for i in range(2)]; r_sgt = [R() for _ in range(2)]
        hsh_l = [k.sbuf("hsh%d" % i, [128, 2, GN], BF16, ph) for i in range(2)]; r_hsh_l = [R() for _ in range(2)]
        junk4 = k.sbuf("junk4", [128, D], BF16, ph); r_junk4 = R()
        yT_v = yT_s.rearrange("(k p) s -> p k s", p=128)
        scr = k.sbuf("scr", [128, 2 * NE], F32, ph); r_scr = R()
        sel = k.sbuf("sel", [128, 2 * NE], F32, ph); r_sel = R()
        selm = k.sbuf("selm", [128, 2 * NE], F32, ph); r_selm = R()
        eq = k.sbuf("eq", [128, 2 * NE], F32, ph); r_eq = R()
        Mb = k.sbuf("Mb", [128, 2 * NE], BF16, ph); r_Mb = R()
        Wgt = k.sbuf("Wgt", [128, 2 * NE], F32, ph); r_Wgt = R()
        oh = k.sbuf("oh", [128, 8 * NE], F32, ph); r_oh = R()
        g8 = k.sbuf("g8", [128, 3, 16], F32, ph); r_g8 = R()
        t8 = k.sbuf("t8", [128, 16], F32, ph); r_t8 = R()
        i8u = k.sbuf("i8u", [128, 16], U32, ph); r_i8u = R()
        wsum = k.sbuf("wsum", [128, 8], F32, ph); r_wsum = R()
        k.MS("pool", cnt_run[:], 0.0, [r_cnt])
        k.MS("pool", W8[:], 0.0, [r_W8])
        xti_c = [0]

        def bufs(g):
            return (x1_l[g % 4], r_x1_l[g % 4], xs2_l[g % 2], r_xs2_l[g % 2], fT32_l[g % 2], r_fT32_l[g % 2],
                    yTg_l[g % 2], r_yTg_l[g % 2], fTb_l[g % 2], r_fTb_l[g % 2])

        NG4 = S // GN

        def loads4(g):
            if g >= S // GN:
                return
            k.DMA("sp", yTg_l[g % 2][:], yT_v[:, :, g * GN:(g + 1) * GN], reads=[r_yTs], writes=[r_yTg_l[g % 2]])
            for ti in range(GT):
                tok0 = g * GN + ti * 128
                b = (g * GT + ti) % 4
                k.DMA("sp", xt[b][:], x_d[tok0:tok0 + 128, :], writes=[r_xt[b]])

        loads4(0)

        def stageA(g):
            x1, r_x1, xs2, r_xs2, fT32, r_fT32, yTg, r_yTg, fTb, r_fTb = bufs(g)
            loads4(g + 1)
            for ti in range(GT):
                tok0 = g * GN + ti * 128
                b = (g * GT + ti) % 4
                rx1 = r_x1[ti]
                for nn in range(2):
                    po, rpo = PB[(ti * 2 + nn) % 4], RPB[(ti * 2 + nn) % 4]
                    for kk in range(8):
                        k.MM(po[:, :], yTg[:, kk, ti * 128:(ti + 1) * 128], wout[:, kk, nn * 512:(nn + 1) * 512], kk == 0, kk == 7,
                             [r_yTg, r_wout], [rpo])
                    k.TT("dve", x1[:, ti, nn * 512:(nn + 1) * 512], po[:, :], G1b[:, nn * 512:(nn + 1) * 512], ALU.mult,
                         [rpo, r_G1b], [rx1])
                k.TT("dve", x1[:, ti, :], x1[:, ti, :], xt[b][:], ALU.add, [rx1, r_xt[b]], [rx1])
                ss, rss = new_stat()
                k.ACT(junk4[:], x1[:, ti, :], AF.Square, [rx1], [r_junk4, rss], accum=ss)
                rstd, rrs = rstd_from(ss, rss, D)
                k.ACT(xs2[:, ti, :], x1[:, ti, :], AF.Copy, [rx1, rrs], [r_xs2[ti]], scale=rstd)
                fb_, rfb_ = ftk[ti % 2], r_ftk[ti % 2]
                k.TT("dve", tmpf2[:], xs2[:, ti, :], A2b[:], ALU.mult, [r_xs2[ti], r_A2b], [r_tmpf2])
                k.TT("dve", fb_[:], tmpf2[:], B2b[:], ALU.add, [r_tmpf2, r_B2b], [rfb_])
                k.DMA("sp", ftok_s[tok0:tok0 + 128, :], fb_[:], reads=[rfb_])

        def stageA2(g):
            x1, r_x1, xs2, r_xs2, fT32, r_fT32, yTg, r_yTg, fTb, r_fTb = bufs(g)
            for kk in range(8):
                pb, rpb = PB[4 + kk % 2], RPB[4 + kk % 2]
                for ti in range(GT):
                    k.TR(pb[:, ti * 128:(ti + 1) * 128], xs2[:, ti, kk * 128:(kk + 1) * 128], ident_f[:],
                         [r_xs2[ti], r_idf], [rpb])
                if kk % 2 == 0:
                    k.TS("dve", fT32[:, kk, :], pb[:, 0:GN], A2[:, kk:kk + 1], modT[:, 24 + kk, 0:1], ALU.mult, ALU.add,
                         [rpb, r_A2, r_modT], [r_fT32[kk]])
                else:
                    k.ACT(fT32[:, kk, :], pb[:, 0:GN], AF.Identity, [rpb, r_A2, r_modT], [r_fT32[kk]],
                          bias=modT[:, 24 + kk, 0:1], scale=A2[:, kk:kk + 1])
                k.CP("pool", fTb[:, kk, :], fT32[:, kk, :], [r_fT32[kk]], [r_fTb[kk]])
            pr, rpr = PB[6], RPB[6]
            pk, rpk = PB[7], RPB[7]
            for ti in range(GT):
                for kk in range(8):
                    k.MM(pr[:, ti * NE:(ti + 1) * NE], fT32[:, kk, ti * 128:(ti + 1) * 128], rw[:, kk, :], kk == 0, kk == 7,
                         [r_fT32[kk], r_rw], [rpr])
            k.ACT(scr[:], pr[:, 0:GT * NE], AF.Sigmoid, [rpr], [r_scr])
            scr3 = scr[:].rearrange("p (t e) -> p t e", e=NE)
            sel3t = sel[:].rearrange("p (t e) -> p t e", e=NE)
            k.TT("dve", sel3t, scr3, rbias[:].unsqueeze(1).to_broadcast([128, GT, NE]), ALU.add, [r_scr, r_rbias], [r_sel])
            selg = sel[:].rearrange("p (a b) -> p a b", b=8)
            eqg = eq[:].rearrange("p (a b) -> p a b", b=8)
            selmg = selm[:].rearrange("p (a b) -> p a b", b=8)
            k.op("dve", (lambda o=g8[:, 0, :], i=selg: (lambda e: e.tensor_reduce(out=o, in_=i, axis=AX.X, op=ALU.max)))(),
                 [r_sel], [r_g8])
            k.TT("dve", eqg, selg, g8[:, 0, :].unsqueeze(2).to_broadcast([128, GT * 8, 8]), ALU.is_equal, [r_sel, r_g8], [r_eq])
            k.STT(selm[:], eq[:], -1e9, sel[:], ALU.mult, ALU.add, [r_eq, r_sel], [r_selm])
            k.op("dve", (lambda o=g8[:, 1, :], i=selmg: (lambda e: e.tensor_reduce(out=o, in_=i, axis=AX.X, op=ALU.max)))(),
                 [r_selm], [r_g8])
            k.TT("dve", g8[:, 1, :], g8[:, 1, :], g8[:, 0, :], ALU.add, [r_g8], [r_g8])
            gs3 = g8[:, 1, :].rearrange("p (t b) -> p t b", b=8)
            t8v = t8[:].rearrange("p (t b) -> p t b", b=8)
            for ti in range(GT):
                k.op("dve", (lambda o=t8[:, ti * 8:(ti + 1) * 8], i=g8[:, 1, ti * 8:(ti + 1) * 8]: (lambda e: e.max(out=o, in_=i)))(),
                     [r_g8], [r_t8])
            pen3 = g8[:, 2, :].rearrange("p (t b) -> p t b", b=8)
            k.TT("dve", pen3, gs3, t8v[:, :, 3:4].to_broadcast([128, GT, 8]), ALU.is_ge, [r_g8, r_t8], [r_g8])
            k.TS("dve", g8[:, 2, :], g8[:, 2, :], 1.0, 1e9, ALU.subtract, ALU.mult, [r_g8], [r_g8])
            k.TT("dve", selmg, selg, g8[:, 2, :].unsqueeze(2).to_broadcast([128, GT * 8, 8]), ALU.add, [r_sel, r_g8], [r_selm])
            for ti in range(GT):
                k.op("dve", (lambda o=t8[:, ti * 8:(ti + 1) * 8], i=selm[:, ti * NE:(ti + 1) * NE]: (lambda e: e.max(out=o, in_=i)))(),
                     [r_selm], [r_t8])
                k.op("dve", (lambda o=i8u[:, ti * 8:(ti + 1) * 8], m=t8[:, ti * 8:(ti + 1) * 8], v=selm[:, ti * NE:(ti + 1) * NE]:
                             (lambda e: e.max_index(out=o, in_max=m, in_values=v)))(),
                     [r_selm, r_t8], [r_i8u])
            e8g = eidx8f[:, g * GT:(g + 1) * GT, :]
            k.CP("dve", e8g, i8u[:].rearrange("p (t b) -> p t b", b=8), [r_i8u], [r_eidx8f])
            selm3t = selm[:].rearrange("p (t e) -> p t e", e=NE)
            eq3t = eq[:].rearrange("p (t e) -> p t e", e=NE)
            k.TT("dve", eq3t, selm3t, t8v[:, :, 7:8].to_broadcast([128, GT, NE]), ALU.is_ge, [r_selm, r_t8], [r_eq])
            k.CP("pool", Mb[:], eq[:], [r_eq], [r_Mb])
            k.TT("dve", Wgt[:], eq[:], scr[:], ALU.mult, [r_eq, r_scr], [r_Wgt])
            Wgt3 = Wgt[:].rearrange("p (t e) -> p t e", e=NE)
            k.op("dve", (lambda o=wsum[:, 0:GT], i=Wgt3: (lambda e: e.tensor_reduce(out=o, in_=i, axis=AX.X, op=ALU.add)))(),
                 [r_Wgt], [r_wsum])
            k.RCP(wsum[:, 4:4 + GT], wsum[:, 0:GT], [r_wsum], [r_wsum])
            k.TS("dve", wsum[:, 4:4 + GT], wsum[:, 4:4 + GT], 2.5, None, ALU.mult, None, [r_wsum], [r_wsum])
            k.TT("dve", Wgt3, Wgt3, wsum[:, 4:4 + GT].unsqueeze(2).to_broadcast([128, GT, NE]), ALU.mult, [r_Wgt, r_wsum], [r_Wgt])
            for ti in range(GT):
                oh3 = oh[:].rearrange("p (j e) -> p j e", e=NE)
                k.TT("dve", oh3, iota64[:].unsqueeze(1).to_broadcast([128, 8, NE]),
                     eidx8f[:, g * GT + ti, :].unsqueeze(2).to_broadcast([128, 8, NE]), ALU.is_equal, [r_iota, r_eidx8f], [r_oh])
                k.TT("dve", oh3, oh3, Wgt[:, ti * NE:(ti + 1) * NE].unsqueeze(1).to_broadcast([128, 8, NE]), ALU.mult,
                     [r_oh, r_Wgt], [r_oh])
                k.op("dve", (lambda o=W8[:, g * GT + ti, :], i=oh3: (lambda e: e.tensor_reduce(out=o, in_=i, axis=AX.X, op=ALU.add)))(),
                     [r_oh], [r_W8])

        def stageA2b(g):
            pk, rpk = PB[7], RPB[7]
            for ti in range(GT):
                nacc = ti + 1
                k.MM(pk[:, ti * NE:(ti + 1) * NE], tri_bf[:], Mb[:, ti * NE:(ti + 1) * NE], True, nacc == 1, [r_tri, r_Mb], [rpk])
                for tj in range(ti):
                    k.MM(pk[:, ti * NE:(ti + 1) * NE], ones_bf[:], Mb[:, tj * NE:(tj + 1) * NE], False, tj == ti - 1,
                         [r_onesb, r_Mb], [rpk])
            for ti in range(GT):
                k.MM(pk[:, 4 * NE:5 * NE], ones_bf[:], Mb[:, ti * NE:(ti + 1) * NE], ti == 0, ti == GT - 1, [r_onesb, r_Mb], [rpk])
            k.TT("dve", rkc[:, g * GT:(g + 1) * GT, :], pk[:, 0:GT * NE].rearrange("p (t e) -> p t e", e=NE),
                 cnt_run[:].unsqueeze(1).to_broadcast([128, GT, NE]), ALU.add, [rpk, r_cnt], [r_rkc])
            k.TT("dve", cnt_run[:], pk[:, 4 * NE:5 * NE], cnt_run[:], ALU.add, [rpk, r_cnt], [r_cnt])

        def stageB(g):
            x1, r_x1, xs2, r_xs2, fT32, r_fT32, yTg, r_yTg, fTb, r_fTb = bufs(g)
            hsh, r_hsh = hsh_l[g % 2], r_hsh_l[g % 2]
            for fc in range(2):
                pg, rpg = PB[fc * 2], RPB[fc * 2]
                pu, rpu = PB[fc * 2 + 1], RPB[fc * 2 + 1]
                for kk in range(8):
                    k.MM(pg[:, 0:GN], wsh[:, kk * 256 + fc * 128:kk * 256 + fc * 128 + 128], fTb[:, kk, :], kk == 0, kk == 7,
                         [r_wsh, r_fTb[kk]], [rpg])
                for kk in range(8):
                    k.MM(pu[:, 0:GN], wsh[:, 2048 + kk * 256 + fc * 128:2048 + kk * 256 + fc * 128 + 128], fTb[:, kk, :],
                         kk == 0, kk == 7, [r_wsh, r_fTb[kk]], [rpu])
                k.ACT(sgt[fc][:], pg[:, 0:GN], AF.Silu, [rpg], [r_sgt[fc]])
                k.TT("dve", hsh[:, fc, :], pu[:, 0:GN], sgt[fc][:], ALU.mult, [rpu, r_sgt[fc]], [r_hsh])

        def stageB2(g):
            x1, r_x1, xs2, r_xs2, fT32, r_fT32, yTg, r_yTg, fTb, r_fTb = bufs(g)
            hsh, r_hsh = hsh_l[g % 2], r_hsh_l[g % 2]
            for ti in range(GT):
                tok0 = g * GN + ti * 128
                rx1 = r_x1[ti]
                for nn in range(2):
                    pd, rpd = PB[4 + nn], RPB[4 + nn]
                    for fc in range(2):
                        k.MM(pd[:, :], hsh[:, fc, ti * 128:(ti + 1) * 128],
                             wsh[:, 4096 + fc * 1024 + nn * 512:4096 + fc * 1024 + nn * 512 + 512],
                             fc == 0, fc == 1, [r_hsh, r_wsh], [rpd])
                    k.TT("dve", tmpf[:, nn * 512:(nn + 1) * 512], pd[:, :], G2b[:, nn * 512:(nn + 1) * 512], ALU.mult,
                         [rpd, r_G2b], [r_tmpf_h[nn]])
                    k.TT("dve", x1[:, ti, nn * 512:(nn + 1) * 512], x1[:, ti, nn * 512:(nn + 1) * 512],
                         tmpf[:, nn * 512:(nn + 1) * 512], ALU.add, [rx1, r_tmpf_h[nn]], [rx1])
                k.DMA("sp", x1_s[tok0:tok0 + 128, :], x1[:, ti, :], reads=[rx1])

        NG4 = S // GN
        for g in range(NG4 + 3):
            if g < NG4:
                stageA(g)
            if 1 <= g <= NG4:
                stageA2(g - 1)
            if 2 <= g <= NG4 + 1:
                stageB(g - 2)
            if 1 <= g <= NG4:
                stageA2b(g - 1)
            if g >= 3:
                stageB2(g - 3)
        k.barrier()

    with contextlib.ExitStack() as ph:
        pad = k.sbuf("pad", [128, NE], F32, ph); r_pad = R()
        padi = k.sbuf("padi", [128, NE], I32, ph); r_padi = R()
        pend = k.sbuf("pend", [128, NE], F32, ph); r_pend = R()
        pstart = k.sbuf("pstart", [128, NE], F32, ph); r_pstart = R()
        ones64 = k.sbuf("ones64", [128, NE], F32, ph); r_o64 = R()
        cmp = k.sbuf("cmp", [128, 64, NE], F32, ph); r_cmp = R()
        blkE = k.sbuf("blkE", [128, NBLK], F32, ph); r_blkE = R()
        bst = k.sbuf("bst", [128, NBLK], F32, ph); r_bst = R()
        k.DMA("sp", bst[:], bstart_d, writes=[r_bst])
        pidx = k.sbuf("pidx_sb", [128, 1], F32, ph); r_pidx = R()
        k.DMA("sp", pidx[:], pidx_d, writes=[r_pidx])
        k.MS("pool", ones64[:], 1.0, [r_o64])
        k.TS("dve", pad[:], cnt_run[:], 127.0, None, ALU.add, None, [r_cnt], [r_pad])
        k.CP("dve", padi[:], pad[:], [r_pad], [r_padi])
        k.TS("dve", padi[:], padi[:], 7, 7, ALU.arith_shift_right, ALU.logical_shift_left, [r_padi], [r_padi])
        k.CP("dve", pad[:], padi[:], [r_padi], [r_pad])
        k.op("dve", lambda e: e.tensor_tensor_scan(out=pend[:], data0=ones64[:], data1=pad[:], initial=0.0,
                                                   op0=ALU.mult, op1=ALU.add), [r_o64, r_pad], [r_pend])
        k.TT("dve", pstart[:], pend[:], pad[:], ALU.subtract, [r_pend, r_pad], [r_pstart])
        for c0 in range(0, NBLK, 64):
            k.TT("dve", cmp[:], pend[:].unsqueeze(1).to_broadcast([128, 64, NE]),
                 bst[:, c0:c0 + 64].unsqueeze(2).to_broadcast([128, 64, NE]), ALU.is_le, [r_pend, r_bst], [r_cmp])
            k.op("dve", (lambda o=blkE[:, c0:c0 + 64], i=cmp[:]: (lambda e: e.tensor_reduce(out=o, in_=i, axis=AX.X, op=ALU.add)))(),
                 [r_cmp], [r_blkE])
        k.TS("dve", blkE[:], blkE[:], 63.0, None, ALU.min, None, [r_blkE], [r_blkE])
        same = k.sbuf("same", [128, NBLK], F32, ph); r_same = R()
        k.MS("dve", same[:], 0.0, [r_same])
        k.TT("dve", same[:, 1:NBLK], blkE[:, 1:NBLK], blkE[:, 0:NBLK - 1], ALU.is_equal, [r_blkE], [r_same])
        for q in range(1, NWB6):
            k.MS("dve", same[:, q * RUN6:q * RUN6 + 1], 0.0, [r_same])
        k.TS("dve", blkE[:], blkE[:], 128.0, pidx[:, 0:1], ALU.mult, ALU.add, [r_blkE, r_pidx], [r_blkE])
        k.STT(blkE[:], same[:], float(1 << 20), blkE[:], ALU.mult, ALU.add, [r_same, r_blkE], [r_blkE])
        k.CP("dve", idxW[:], blkE[:], [r_blkE], [r_idxW])
        destf = [k.sbuf("destf%d" % i, [128, NE], F32, ph) for i in range(2)]; r_destf = [R() for _ in range(2)]
        d8 = [k.sbuf("d8%d" % i, [128, 8], F32, ph) for i in range(2)]; r_d8 = [R() for _ in range(2)]
        j64b = k.sbuf("j64b", [128, NE], F32, ph); r_j64b = R()
        ft = [k.sbuf("ft%d" % i, [128, D], BF16, ph) for i in range(3)]; r_ft = [R() for _ in range(3)]
        for i in range(32):
            df, rdf = destf[i % 2], r_destf[i % 2]
            dd, rdd = d8[i % 2], r_d8[i % 2]
            k.TT("dve", df[:], rkc[:, i, :], pstart[:], ALU.add, [r_rkc, r_pstart], [rdf])
            k.MS("dve", dd[:], 0.0, [rdd])
            for j in range(8):
                k.op("dve", (lambda o=j64b[:], i0=iota64[:], sc_=eidx8f[:, i, j:j + 1], i1=df[:], a=dd[:, j:j + 1]:
                             (lambda e: e.scalar_tensor_tensor(out=o, in0=i0, scalar=sc_, in1=i1, op0=ALU.is_equal,
                                                               op1=ALU.mult, accum_out=a)))(),
                     [r_iota, r_eidx8f, rdf], [r_j64b, rdd])
            k.CP("dve", dest8u[:, i, :], dd[:], [rdd], [r_dest8u[i]])
            fb_, rfb_ = ft[i % 3], r_ft[i % 3]
            k.DMA("sp", fb_[:], ftok_s[i * 128:(i + 1) * 128, :], reads=[r_ftoks], writes=[rfb_])
            for j in range(8):
                k.dma("pool", (lambda e, src=fb_[:], idx=dest8u[:, i, j:j + 1]:
                               e.indirect_dma_start(out=Xs[:, :], out_offset=bass.IndirectOffsetOnAxis(ap=idx, axis=0),
                                                    in_=src, in_offset=None)),
                      reads=[rfb_, r_dest8u[i]])
        k.barrier()

    with contextlib.ExitStack() as ph:
        NW = NWB6
        wblk = [k.sbuf("wblk%d" % i, [128, 6144], BF16, ph) for i in range(NW)]; r_wblk = [R() for _ in range(NW)]
        xblk = [k.sbuf("xblk%d" % i, [128, D], BF16, ph) for i in range(3)]; r_xblk = [R() for _ in range(3)]
        xT = [k.sbuf("xTb%d" % i, [128, D], BF16, ph) for i in range(2)]; r_xT = [R() for _ in range(2)]
        sgb = [k.sbuf("sgb%d" % i, [128, 256], F32, ph) for i in range(2)]; r_sgb = [R() for _ in range(2)]
        hidb = [k.sbuf("hidb%d" % i, [128, 256], BF16, ph) for i in range(2)]; r_hidb = [R() for _ in range(2)]
        yblk = [k.sbuf("yblk%d" % i, [128, D], F32, ph) for i in range(2)]; r_yblk = [R() for _ in range(2)]

        def sig(b):
            return (b % NW) * RUN6 + b // NW

        bcreg = [None]

        def gather_w(e, o, idx):
            if bcreg[0] is None:
                bcreg[0] = e.alloc_register("bcr")
                e.reg_mov(bcreg[0], NE * 128 - 1)
            return e.indirect_dma_start(out=o, out_offset=None, in_=W_all[:, :],
                                        in_offset=bass.IndirectOffsetOnAxis(ap=idx, axis=0),
                                        bounds_check=bcreg[0], oob_is_err=False)

        def load_w(b):
            wb = b % NW
            k.dma("pool", (lambda e, o=wblk[wb][:], idx=idxW[:, sig(b):sig(b) + 1]: gather_w(e, o, idx)),
                  reads=[r_Wall, r_idxW], writes=[r_wblk[wb]])

        def load_x(b):
            k.DMA("sp", xblk[b % 3][:], Xs[sig(b) * 128:(sig(b) + 1) * 128, :], reads=[r_Xs], writes=[r_xblk[b % 3]])

        hidT = [k.sbuf("hidT%d" % i, [128, 256], BF16, ph) for i in range(2)]; r_hidT = [R() for _ in range(2)]

        def TRb(b):
            pb, rpb = PB[b % 2], RPB[b % 2]
            pbv = pb[:].bitcast(BF16)
            for kk in range(8):
                k.TR(pbv[:, kk * 128:(kk + 1) * 128], xblk[b % 3][:, kk * 128:(kk + 1) * 128], ident_bf[:],
                     [r_xblk[b % 3], r_idb], [rpb])
            if b % 2 == 0:
                k.CP("act", xT[b % 2][:], pbv[:, :], [rpb], [r_xT[b % 2]])
            else:
                k.CP("dve", xT[b % 2][:], pbv[:, :], [rpb], [r_xT[b % 2]])

        def GUb(b):
            wb = b % NW
            pg, rpg = PB[2 + b % 2], RPB[2 + b % 2]
            for kk in range(8):
                k.MM(pg[:, :], xT[b % 2][:, kk * 128:(kk + 1) * 128], wblk[wb][:, kk * 512:(kk + 1) * 512],
                     kk == 0, kk == 7, [r_wblk[wb], r_xT[b % 2]], [rpg])
            k.ACT(sgb[b % 2][:], pg[:, 0:256], AF.Silu, [rpg], [r_sgb[b % 2]])
            k.TT("dve", hidb[b % 2][:], pg[:, 256:512], sgb[b % 2][:], ALU.mult, [rpg, r_sgb[b % 2]], [r_hidb[b % 2]])

        def HTb(b):
            pg, rpg = PB[2 + b % 2], RPB[2 + b % 2]
            pgv = pg[:].bitcast(BF16)
            for fc in range(2):
                k.TR(pgv[:, fc * 128:(fc + 1) * 128], hidb[b % 2][:, fc * 128:(fc + 1) * 128], ident_bf[:],
                     [r_hidb[b % 2], r_idb], [rpg])
            if b % 2 == 0:
                k.CP("dve", hidT[b % 2][:], pgv[:, 0:256], [rpg], [r_hidT[b % 2]])
            else:
                k.CP("act", hidT[b % 2][:], pgv[:, 0:256], [rpg], [r_hidT[b % 2]])

        def DNb(b):
            wb = b % NW
            for nn in range(2):
                pd, rpd = PB[4 + (b % 2) * 2 + nn], RPB[4 + (b % 2) * 2 + nn]
                for fc in range(2):
                    k.MM(pd[:, :], hidT[b % 2][:, fc * 128:(fc + 1) * 128],
                         wblk[wb][:, 4096 + fc * 1024 + nn * 512:4096 + fc * 1024 + nn * 512 + 512],
                         fc == 0, fc == 1, [r_hidT[b % 2], r_wblk[wb]], [rpd])
                if nn == 0:
                    k.CP("act", yblk[b % 2][:, 0:512], pd[:, :], [rpd], [r_yblk[b % 2]])
                else:
                    k.CP("dve", yblk[b % 2][:, 512:1024], pd[:, :], [rpd], [r_yblk[b % 2]])
            k.DMA("sp", Ys[sig(b) * 128:(sig(b) + 1) * 128, :], yblk[b % 2][:], reads=[r_yblk[b % 2]])

        load_x(0)
        load_x(1)
        load_w(0)
        for i in range(NBLK + 3):
            if i + 2 < NBLK:
                load_x(i + 2)
            if i + 1 < NBLK:
                load_w(i + 1)
            if i < NBLK:
                TRb(i)
            if 1 <= i <= NBLK:
                GUb(i - 1)
            if 2 <= i <= NBLK + 1:
                HTb(i - 2)
            if i >= 3:
                DNb(i - 3)
        k.barrier()

    out_toks = []
    with contextlib.ExitStack() as ph:
        fng = k.sbuf("fng", [128, D], F32, ph); r_fng = R()
        k.DMA("sp", fng[:], fng_d, writes=[r_fng])
        yg = [k.sbuf("yg%d" % i, [128, D], F32, ph) for i in range(8)]; r_yg = [R() for _ in range(8)]
        accb = [k.sbuf("accb%d" % i, [128, D], F32, ph) for i in range(2)]; r_accb = [R() for _ in range(2)]
        acp = [k.sbuf("acp%d" % i, [128, D], F32, ph) for i in range(2)]; r_acp = [R() for _ in range(2)]
        xt = [k.sbuf("xt7%d" % i, [128, D], F32, ph) for i in range(2)]; r_xt = [R() for _ in range(2)]
        ot = [k.sbuf("ot7%d" % i, [128, D], F32, ph) for i in range(2)]; r_ot = [R() for _ in range(2)]
        junk5 = k.sbuf("junk7", [128, D], BF16, ph); r_junk5 = R()
        gi_ = 0
        for i in range(32):
            b2 = i % 2
            tok0 = i * 128
            k.DMA("sp", xt[b2][:], x1_s[tok0:tok0 + 128, :], reads=[r_x1s], writes=[r_xt[b2]])
            ac, rac = accb[b2], r_accb[b2]
            for j in range(8):
                gb, rgb = yg[gi_ % 8], r_yg[gi_ % 8]
                gi_ += 1
                k.dma("pool", (lambda e, o=gb[:], idx=dest8u[:, i, j:j + 1]:
                               e.indirect_dma_start(out=o, out_offset=None, in_=Ys[:, :],
                                                    in_offset=bass.IndirectOffsetOnAxis(ap=idx, axis=0))),
                      reads=[r_Ys, r_dest8u[i]], writes=[rgb])
                if j == 0:
                    k.TS("dve", ac[:], gb[:], W8[:, i, 0:1], None, ALU.mult, None, [rgb, r_W8], [rac])
                else:
                    k.STT(ac[:], gb[:], W8[:, i, j:j + 1], ac[:], ALU.mult, ALU.add, [rgb, r_W8, rac], [rac])
            k.TT("dve", ac[:], ac[:], G2b[:], ALU.mult, [rac, r_G2b], [rac])
            k.TT("dve", ac[:], ac[:], xt[b2][:], ALU.add, [rac, r_xt[b2]], [rac])
            ss, rss = new_stat()
            k.ACT(junk5[:], ac[:], AF.Square, [rac], [r_junk5, rss], accum=ss)
            rstd, rrs = rstd_from(ss, rss, D)
            k.STT(ot[b2][:], ac[:], rstd, fng[:], ALU.mult, ALU.mult, [rac, rrs, r_fng], [r_ot[b2]])
            out_toks.append(k.DMA("sp", y_d[tok0:tok0 + 128, :], ot[b2][:], reads=[r_ot[b2]], final=True))

    if "moe" in debug:
        dbg["cnt_run"] = (cnt_run, [128, NE], F32, [r_cnt])
        dbg["eidx8f"] = (eidx8f, [128, 32, 8], F32, [r_eidx8f])
        dbg["W8"] = (W8, [128, 32, 8], F32, [r_W8])
        dbg["dest8u"] = (dest8u, [128, 32, 8], U32, r_dest8u)
        dbg["idxW"] = (idxW, [128, NBLK], U32, [r_idxW])
        dbg["rkc"] = (rkc, [128, 32, NE], F32, [r_rkc])
    dbg_outs = {}
    for name, (tile, shape, dt, rr) in dbg.items():
        dd = nc.dram_tensor("dbg_" + name, list(shape), dt, kind="ExternalOutput").ap()
        out_toks.append(k.DMA("sp", dd, tile[:], reads=rr))
        dbg_outs[name] = "dbg_" + name
    k.barrier()
    k.wait_all("sp", out_toks)
    k.emit()
    k.es.close()
    return nc, dbg_outs


def rope_tables():
    rows = S // 64
    row = np.repeat(np.arange(rows, dtype=np.float32), 64)
    col = np.tile(np.arange(64, dtype=np.float32), rows)
    inv_freq = (10000.0 ** (-np.arange(0, 16, 2, dtype=np.float32) / 16)).astype(np.float32)
    ang = np.stack([row, col], axis=-1)[:, :, None] * inv_freq
    cos = np.cos(ang).astype(np.float32)
    sin = np.sin(ang).astype(np.float32)
    cosT = np.zeros((32, S), np.float32)
    sinT = np.zeros((32, S), np.float32)
    for a in range(2):
        for hf in range(2):
            cosT[a * 16 + hf * 8:a * 16 + hf * 8 + 8, :] = cos[:, a, :].T
            sinT[a * 16 + hf * 8:a * 16 + hf * 8 + 8, :] = sin[:, a, :].T
    return cosT, sinT


def fm(v, nchunks):
    return np.ascontiguousarray(np.asarray(v, np.float32).reshape(nchunks, 128).T)


def make_in_maps(inp):
    g = lambda n: np.asarray(inp[n], dtype=np.float32)
    cosT, sinT = rope_tables()
    shared = {
        "w_mod": np.ascontiguousarray(g("w_mod")[0]),
        "bmT": fm(g("b_mod")[0], 48),
        "bmrow": np.ascontiguousarray(g("b_mod")[0].reshape(1, -1)),
        "nmgT": fm(g("norm_mix_g")[0], 8),
        "nfgT": fm(g("norm_ffn_g")[0], 8),
        "w_in": np.ascontiguousarray(g("w_in")[0]),
        "qngT": fm(g("q_norm_g")[0], 2),
        "w_q_up": np.ascontiguousarray(g("w_q_up")[0]),
        "kvngT": fm(g("kv_norm_g")[0], 1),
        "w_kv_up": np.ascontiguousarray(g("w_kv_up")[0]),
        "cwT": np.ascontiguousarray(g("conv_w")[0].reshape(4, 4, 128).transpose(2, 1, 0)),
        "cbT": fm(g("conv_b")[0], 4),
        "lru_w_a": np.ascontiguousarray(g("lru_w_a")[0]),
        "lru_w_x": np.ascontiguousarray(g("lru_w_x")[0]),
        "lbaT": np.ascontiguousarray(g("lru_b_a")[0].reshape(2, 4, 128).transpose(2, 0, 1)),
        "lbxT": np.ascontiguousarray(g("lru_b_x")[0].reshape(2, 4, 128).transpose(2, 0, 1)),
        "lamT": np.ascontiguousarray(g("lru_lambda")[0].reshape(2, 4, 128).transpose(2, 0, 1)),
        "w_out": np.ascontiguousarray(g("w_out")[0]),
        "router_w": np.ascontiguousarray(g("router_w")[0]),
        "rbias_b": np.ascontiguousarray(np.broadcast_to(g("router_bias")[0][None, :], (128, NE))),
        "exp_w_gate": np.ascontiguousarray(g("exp_w_gate")[0]),
        "exp_w_up": np.ascontiguousarray(g("exp_w_up")[0]),
        "exp_w_down": np.ascontiguousarray(g("exp_w_down")[0]),
        "sh_w_gate": np.ascontiguousarray(g("sh_w_gate")[0]),
        "sh_w_up": np.ascontiguousarray(g("sh_w_up")[0]),
        "sh_w_down": np.ascontiguousarray(g("sh_w_down")[0]),
        "fng_b": np.ascontiguousarray(np.broadcast_to(g("final_norm_g")[None, :], (128, D))),
        "cosT": cosT,
        "sinT": sinT,
        "tri_bf": np.triu(np.ones((128, 128), np.float32), 1).astype(ml_dtypes.bfloat16),
        "iota64": np.ascontiguousarray(np.broadcast_to(np.arange(NE, dtype=np.float32)[None, :], (128, NE))),
        "bstart": np.ascontiguousarray(np.broadcast_to((128.0 * np.arange(NBLK, dtype=np.float32))[None, :], (128, NBLK))),
        "pidx": np.arange(128, dtype=np.float32).reshape(128, 1),
        "nfg_b": np.ascontiguousarray(np.broadcast_to(g("norm_ffn_g")[0][None, :], (128, D))),
        "ident_bf": np.eye(128, dtype=np.float32).astype(ml_dtypes.bfloat16),
        "ident_f": np.eye(128, dtype=np.float32),
    }
    x = g("x")
    c = g("c")
    ctx = g("ctx")
    c_ctx = g("c_ctx")
    maps = []
    for b in range(8):
        m = dict(shared)
        m["x"] = np.ascontiguousarray(x[b])
        m["ctx"] = np.ascontiguousarray(ctx[b])
        cT = np.stack([c[b].reshape(8, 128).T, c_ctx.reshape(8, 128).T], axis=-1)
        m["cT"] = np.ascontiguousarray(cT)
        maps.append(m)
    return maps


_NC_CACHE = {}


def kernel(**inputs):
    if "nc" not in _NC_CACHE:
        _NC_CACHE["nc"] = build_nc()[0]
    nc = _NC_CACHE["nc"]
    maps = make_in_maps(inputs)
    res = run_bass_kernel_spmd(nc, maps, core_ids=list(range(8)))
    out = np.stack([np.asarray(res.results[b]["y"], dtype=np.float32) for b in range(8)], axis=0)
    return out
```
